# Optimizing a Trainium2 kernel written in Bass

```python
import functools
import jax, jax.numpy as jnp
from jax import lax
import numpy as np

D_MODEL = 1024
BATCH = 8
SEQ = 2048
DEPTH = 1
DEC_BATCH = 32
DEC_SEQ = 1
PAST_LEN = 8192
PAGE_SIZE = 128

MIX_WIDTH = D_MODEL
H_RET = 8
D_RET = MIX_WIDTH // (2 * H_RET)
H_ATT = 8
D_HEAD = MIX_WIDTH // (2 * H_ATT)
RET_W = H_RET * D_RET
ATT_W = H_ATT * D_HEAD
H_IDX = 8
D_IDX = 64
TOPK_MAX = 256
N_EXPERTS = 32
TOP_K = 4
D_FF = D_MODEL
SWIGLU_LIMIT = 7.0
SWIGLU_ALPHA = 1.702
ROPE_THETA = 10000.0
EPS = 1e-6
CHUNK = 128
Q_BLOCK = 128
IN_COLS = 4 * RET_W + 3 * ATT_W + H_IDX * D_IDX + D_IDX + H_IDX

kernel_name = 'hymba_retention_dsa_moe_adaln_step'

F32 = jnp.float32


def rms_norm(x, w):
    xf = x.astype(F32)
    y = xf * lax.rsqrt(jnp.mean(xf * xf, axis=-1, keepdims=True) + EPS)
    return (y * w.astype(F32)).astype(x.dtype)


def rope(x, pos):
    d = x.shape[-1]
    inv = jnp.power(ROPE_THETA, -jnp.arange(0, d, 2, dtype=F32) / d)
    ang = pos.astype(F32)[:, None] * inv[None, :]
    ang = jnp.concatenate([ang, ang], axis=-1)
    shape = (1, pos.shape[0]) + (1,) * (x.ndim - 3) + (d,)
    cos = jnp.cos(ang).reshape(shape)
    sin = jnp.sin(ang).reshape(shape)
    xf = x.astype(F32)
    rot = jnp.concatenate([-xf[..., d // 2:], xf[..., :d // 2]], axis=-1)
    return (xf * cos + rot * sin).astype(x.dtype)


def retention_chunks(q, k, v, s0):
    B, T, H, _ = q.shape
    chunk = CHUNK if T % CHUNK == 0 else T
    n = T // chunk
    log_g = jnp.log1p(-jnp.power(2.0, -5.0 - jnp.arange(H, dtype=F32)))
    idx = jnp.arange(chunk, dtype=F32)
    rel = idx[:, None] - idx[None, :]
    decay = jnp.where(rel[None] >= 0, jnp.exp(jnp.maximum(rel, 0.0)[None] * log_g[:, None, None]), 0.0)
    q_dec = jnp.exp((idx + 1.0)[:, None] * log_g[None, :])
    k_dec = jnp.exp((chunk - 1.0 - idx)[:, None] * log_g[None, :])
    g_chunk = jnp.exp(chunk * log_g)

    def to_chunks(a):
        return a.astype(F32).reshape(B, n, chunk, H, a.shape[-1]).transpose(1, 0, 2, 3, 4)

    def step(S, inp):
        qc, kc, vc = inp
        inner = jnp.einsum('bihd,bjhd->bhij', qc, kc) * decay[None]
        o = (jnp.einsum('bhij,bjhv->bihv', inner, vc)
             + jnp.einsum('bihd,bhdv->bihv', qc, S) * q_dec[None, :, :, None])
        S = S * g_chunk[None, :, None, None] + jnp.einsum('bjhd,bjhv->bhdv', kc * k_dec[None, :, :, None], vc)
        return S, o

    S, o = lax.scan(step, s0.astype(F32), (to_chunks(q), to_chunks(k), to_chunks(v)))
    o = o.transpose(1, 0, 2, 3, 4).reshape(B, T, H, -1)
    return o, S


def retention_group(rq, rk, rv, rg, pos, s0, gn_w):
    B, T, _ = rq.shape
    q = rope(rq.reshape(B, T, H_RET, D_RET), pos)
    k = rope(rk.reshape(B, T, H_RET, D_RET), pos) * (D_RET ** -0.5)
    v = rv.reshape(B, T, H_RET, D_RET)
    o, S = retention_chunks(q, k, v, s0)
    mu = jnp.mean(o, axis=-1, keepdims=True)
    var = jnp.mean((o - mu) ** 2, axis=-1, keepdims=True)
    on = ((o - mu) * lax.rsqrt(var + EPS)).reshape(B, T, RET_W) * gn_w.astype(F32)
    out = jax.nn.silu(rg.astype(F32)) * on
    return out.astype(rq.dtype), S.astype(rq.dtype)


def index_scores(qi, wi, ki):
    s = jnp.einsum('bqhd,bsd->bqhs', qi.astype(F32), ki.astype(F32)) * (D_IDX ** -0.5)
    return jnp.einsum('bqhs,bqh->bqs', jax.nn.relu(s), wi.astype(F32))


def indexer_topk(scores, q_pos, n_keys, k_top):
    key_pos = jnp.arange(n_keys)
    visible = key_pos[None, None, :] <= q_pos[None, :, None]
    scores = jnp.where(visible, scores, -jnp.inf)
    _, idx = lax.top_k(scores, k_top)
    ok = idx <= q_pos[None, :, None]
    return idx, ok


def sparse_attend(q, k_sel, v_sel, ok):
    s = jnp.einsum('bqhd,bqkhd->bqhk', q.astype(F32), k_sel.astype(F32)) * (D_HEAD ** -0.5)
    s = jnp.where(ok[:, :, None, :], s, -jnp.inf)
    p = jax.nn.softmax(s, axis=-1)
    return jnp.einsum('bqhk,bqkhd->bqhd', p, v_sel.astype(F32)).astype(q.dtype)


def dsa_prompt(q, k, v, qi, ki, wi):
    B, T = q.shape[:2]
    k_top = min(TOPK_MAX, T // 4)
    nb = T // Q_BLOCK
    bidx = jnp.arange(B)[:, None, None]

    def blk(a):
        return a.reshape((B, nb, Q_BLOCK) + a.shape[2:]).swapaxes(0, 1)

    def one(inp):
        qb, qib, wib, posb = inp
        sc = index_scores(qib, wib, ki)
        idx, ok = indexer_topk(sc, posb, T, k_top)
        return sparse_attend(qb, k[bidx, idx], v[bidx, idx], ok)

    pos_blocks = jnp.arange(T, dtype=jnp.int32).reshape(nb, Q_BLOCK)
    o = lax.map(one, (blk(q), blk(qi), blk(wi), pos_blocks))
    return o.swapaxes(0, 1).reshape(B, T, ATT_W)


def dsa_sample(q, k, v, qi, ki, wi, cache_k, cache_v, cache_idx_k, page_table):
    DB, T = q.shape[:2]
    P = page_table.shape[1] * PAGE_SIZE
    L = P + T
    k_top = min(TOPK_MAX, L // 4)
    ki_past = cache_idx_k[page_table].reshape(DB, P, D_IDX)
    ki_all = jnp.concatenate([ki_past.astype(ki.dtype), ki], axis=1)
    q_pos = P + jnp.arange(T, dtype=jnp.int32)
    sc = index_scores(qi, wi, ki_all)
    idx, ok = indexer_topk(sc, q_pos, L, k_top)
    bidx = jnp.arange(DB)[:, None, None]
    pidx = jnp.minimum(idx, P - 1)
    phys = page_table[bidx, pidx // PAGE_SIZE]
    off = pidx % PAGE_SIZE
    nidx = jnp.clip(idx - P, 0, T - 1)
    is_new = (idx >= P)[..., None, None]
    k_sel = jnp.where(is_new, k[bidx, nidx], cache_k[phys, off].astype(k.dtype))
    v_sel = jnp.where(is_new, v[bidx, nidx], cache_v[phys, off].astype(v.dtype))
    return sparse_attend(q, k_sel, v_sel, ok).reshape(DB, T, ATT_W)


def moe(x, router_w, router_b, w_gate_up, b_gate_up, w_down, b_down):
    logits = (x @ router_w + router_b).astype(F32)
    top_v, top_i = lax.top_k(logits, TOP_K)
    gates = jax.nn.softmax(top_v, axis=-1)
    combine = jnp.einsum('nk,nke->ne', gates, jax.nn.one_hot(top_i, N_EXPERTS, dtype=F32))

    def expert(acc, inp):
        wgu, bgu, wd, bd, cw = inp
        h = (x @ wgu + bgu).astype(F32)
        gate = jnp.minimum(h[:, :D_FF], SWIGLU_LIMIT)
        up = jnp.clip(h[:, D_FF:], -SWIGLU_LIMIT, SWIGLU_LIMIT)
        act = ((up + 1.0) * gate * jax.nn.sigmoid(SWIGLU_ALPHA * gate)).astype(x.dtype)
        out = (act @ wd + bd).astype(F32)
        return acc + cw[:, None] * out, None

    acc, _ = lax.scan(expert, jnp.zeros(x.shape, F32), (w_gate_up, b_gate_up, w_down, b_down, combine.T))
    return acc.astype(x.dtype)


def run_layer(x, c, pos, attend, ret_s0, ada_w, ada_b, norm1_w, w_in, q_norm_w, k_norm_w,
              idx_k_norm_w, ret_gn_w, w_out, norm2_w, router_w, router_b,
              w_gate_up, b_gate_up, w_down, b_down):
    B, T, D = x.shape
    sh1, sc1, g1, sh2, sc2, g2 = jnp.split(jax.nn.silu(c) @ ada_w + ada_b, 6, axis=-1)
    h = rms_norm(x, norm1_w) * (1.0 + sc1[:, None]) + sh1[:, None]
    z = h @ w_in
    sizes = [RET_W] * 4 + [ATT_W] * 3 + [H_IDX * D_IDX, D_IDX, H_IDX]
    rq, rk, rv, rg, aq, ak, av, iq, ik, iw = jnp.split(z, np.cumsum(sizes)[:-1].tolist(), axis=-1)
    ret_out, ret_state = retention_group(rq, rk, rv, rg, pos, ret_s0, ret_gn_w)
    aq = rope(rms_norm(aq.reshape(B, T, H_ATT, D_HEAD), q_norm_w), pos)
    ak = rope(rms_norm(ak.reshape(B, T, H_ATT, D_HEAD), k_norm_w), pos)
    av = av.reshape(B, T, H_ATT, D_HEAD)
    iq = rope(iq.reshape(B, T, H_IDX, D_IDX), pos)
    ik = rope(rms_norm(ik, idx_k_norm_w), pos)
    iw = iw * (H_IDX ** -0.5)
    att_out = attend(aq, ak, av, iq, ik, iw)
    x = x + g1[:, None] * (jnp.concatenate([ret_out, att_out], axis=-1) @ w_out)
    h = rms_norm(x, norm2_w) * (1.0 + sc2[:, None]) + sh2[:, None]
    ff = moe(h.reshape(B * T, D), router_w, router_b, w_gate_up, b_gate_up, w_down, b_down).reshape(B, T, D)
    x = x + g2[:, None] * ff
    return x, ak, av, ik, ret_state


def setup_inputs(seed: int = 0) -> dict:
    key = jax.random.key(seed)
    ks = jax.random.split(key, 32)
    n_pages = PAST_LEN // PAGE_SIZE
    n_used = DEC_BATCH * n_pages
    n_pool = n_used + n_used // 4

    def nrm(k, shape, scale):
        return jax.random.normal(k, shape, F32) * scale

    page_table = jax.random.permutation(ks[0], n_pool)[:n_used].reshape(DEC_BATCH, n_pages).astype(jnp.int32)
    return {
        'x_prompt': nrm(ks[1], (BATCH, SEQ, D_MODEL), 1.0),
        'x_sample': nrm(ks[2], (DEC_BATCH, DEC_SEQ, D_MODEL), 1.0),
        'cache_k': nrm(ks[3], (DEPTH, n_pool, PAGE_SIZE, H_ATT, D_HEAD), 1.0),
        'cache_v': nrm(ks[4], (DEPTH, n_pool, PAGE_SIZE, H_ATT, D_HEAD), 1.0),
        'cache_idx_k': nrm(ks[5], (DEPTH, n_pool, PAGE_SIZE, D_IDX), 1.0),
        'state_ret': nrm(ks[6], (DEPTH, DEC_BATCH, H_RET, D_RET, D_RET), 1.0),
        'page_table': page_table,
        'c_prompt': nrm(ks[7], (BATCH, D_MODEL), 1.0),
        'c_sample': nrm(ks[8], (DEC_BATCH, D_MODEL), 1.0),
        'ada_w': nrm(ks[9], (DEPTH, D_MODEL, 6 * D_MODEL), 0.5 * D_MODEL ** -0.5),
        'ada_b': nrm(ks[10], (DEPTH, 6 * D_MODEL), 0.02),
        'norm1_w': 1.0 + nrm(ks[11], (DEPTH, D_MODEL), 0.02),
        'w_in': nrm(ks[12], (DEPTH, D_MODEL, IN_COLS), D_MODEL ** -0.5),
        'q_norm_w': 1.0 + nrm(ks[13], (DEPTH, D_HEAD), 0.02),
        'k_norm_w': 1.0 + nrm(ks[14], (DEPTH, D_HEAD), 0.02),
        'idx_k_norm_w': 1.0 + nrm(ks[15], (DEPTH, D_IDX), 0.02),
        'ret_gn_w': 1.0 + nrm(ks[16], (DEPTH, RET_W), 0.02),
        'w_out': nrm(ks[17], (DEPTH, MIX_WIDTH, D_MODEL), MIX_WIDTH ** -0.5),
        'norm2_w': 1.0 + nrm(ks[18], (DEPTH, D_MODEL), 0.02),
        'router_w': nrm(ks[19], (DEPTH, D_MODEL, N_EXPERTS), D_MODEL ** -0.5),
        'router_b': nrm(ks[20], (DEPTH, N_EXPERTS), 0.01),
        'w_gate_up': nrm(ks[21], (DEPTH, N_EXPERTS, D_MODEL, 2 * D_FF), D_MODEL ** -0.5),
        'b_gate_up': nrm(ks[22], (DEPTH, N_EXPERTS, 2 * D_FF), 0.01),
        'w_down': nrm(ks[23], (DEPTH, N_EXPERTS, D_FF, D_MODEL), D_FF ** -0.5),
        'b_down': nrm(ks[24], (DEPTH, N_EXPERTS, D_MODEL), 0.01),
    }


def reference(x_prompt, x_sample, cache_k, cache_v, cache_idx_k, state_ret, page_table,
              c_prompt, c_sample, ada_w, ada_b, norm1_w, w_in, q_norm_w, k_norm_w,
              idx_k_norm_w, ret_gn_w, w_out, norm2_w, router_w, router_b,
              w_gate_up, b_gate_up, w_down, b_down):
    B, T = x_prompt.shape[:2]
    DB, TS = x_sample.shape[:2]
    past = page_table.shape[1] * PAGE_SIZE
    pos_p = jnp.arange(T, dtype=jnp.int32)
    pos_s = past + jnp.arange(TS, dtype=jnp.int32)
    hp, hs = x_prompt, x_sample
    kp, vp, ikp, rp, ksl, vsl, iks, rs = [], [], [], [], [], [], [], []
    for l in range(DEPTH):
        lw = (ada_w[l], ada_b[l], norm1_w[l], w_in[l], q_norm_w[l], k_norm_w[l], idx_k_norm_w[l],
              ret_gn_w[l], w_out[l], norm2_w[l], router_w[l], router_b[l],
              w_gate_up[l], b_gate_up[l], w_down[l], b_down[l])
        s0 = jnp.zeros((B, H_RET, D_RET, D_RET), x_prompt.dtype)
        hp, a_k, a_v, i_k, r_s = run_layer(hp, c_prompt, pos_p, dsa_prompt, s0, *lw)
        kp.append(a_k); vp.append(a_v); ikp.append(i_k); rp.append(r_s)
        attend_s = functools.partial(dsa_sample, cache_k=cache_k[l], cache_v=cache_v[l],
                                     cache_idx_k=cache_idx_k[l], page_table=page_table)
        hs, a_k, a_v, i_k, r_s = run_layer(hs, c_sample, pos_s, attend_s, state_ret[l], *lw)
        ksl.append(a_k); vsl.append(a_v); iks.append(i_k); rs.append(r_s)
    k_prompt = jnp.stack(kp)
    v_prompt = jnp.stack(vp)
    idxk_prompt = jnp.stack(ikp)
    ret_prompt = jnp.stack(rp)
    k_sample = jnp.stack(ksl)
    v_sample = jnp.stack(vsl)
    idxk_sample = jnp.stack(iks)
    ret_sample = jnp.stack(rs)
    return (hp, hs, k_prompt, v_prompt, idxk_prompt, ret_prompt, k_sample, v_sample, idxk_sample, ret_sample)
```

```python
import contextlib
import math
import os
import numpy as np
import concourse.bass as bass
import concourse.mybir as mybir
from concourse.bass_utils import run_bass_kernel_spmd

F32 = mybir.dt.float32
BF16 = mybir.dt.bfloat16
I32 = mybir.dt.int32
AF = mybir.ActivationFunctionType
ALU = mybir.AluOpType
AX = mybir.AxisListType

NCORES = 8
D = 1024
SEQ = 2048
NT = SEQ // 128
NS = 4
NPAGES = 64
DBG = int(os.environ.get("MKDBG", "0"))
NPOOL_PAGES = 2560
IN_COLS = 4168
EPS = 1e-6
BIG = 1.0e30
REPL = -1.0e30
CMASK = -3.0e30
NE = 32
CUT = 99
NEXP = int(os.environ.get("NEXP", "32"))
SAMPLE_ON = int(os.environ.get("SAMPLE_ON", "1"))
CUTB = int(os.environ.get("CUTB", "99"))
STAGE = 99


class Prog:
    ENGS = ("pe", "act", "dve", "pool", "sp")

    def __init__(self, nc):
        self.nc = nc
        self.sem = {}
        self.cnt = {}
        self.reset()

    def reset(self):
        self.ops = []
        self.last_w = {}
        self.readers = {}

    def S(self, key):
        if key not in self.sem:
            self.sem[key] = self.nc.alloc_semaphore(name="s%d" % len(self.sem))
        return self.sem[key]

    def add(self, eng, fn, r=(), w=(), dma=None, ndma=1):
        idx = len(self.ops)
        deps = set()
        for k in r:
            if k in self.last_w:
                deps.add(self.last_w[k])
        for k in w:
            if k in self.last_w:
                deps.add(self.last_w[k])
            for x in self.readers.get(k, ()):
                deps.add(x)
        for k in r:
            self.readers.setdefault(k, []).append(idx)
        for k in w:
            self.last_w[k] = idx
            self.readers[k] = []
        deps.discard(idx)
        self.ops.append(dict(eng=eng, fn=fn, deps=deps, dma=dma, ndma=ndma))
        return idx

    def emit(self, last=False):
        ops = self.ops
        start_vals = dict(self.cnt)
        needed = [False] * len(ops)
        for o in ops:
            for d in o["deps"]:
                od = ops[d]
                if od["dma"] is None and o["dma"] is None and od["eng"] == "pe" and o["eng"] == "pe":
                    continue
                needed[d] = True
        cnt = self.cnt
        for i, o in enumerate(ops):
            if o["dma"] is not None:
                key = "dma:" + str(o["dma"])
                cnt[key] = cnt.get(key, 0) + 16 * o["ndma"]
                o["sem"], o["val"], o["signal"] = key, cnt[key], True
            else:
                key = "eng:" + o["eng"]
                if needed[i]:
                    cnt[key] = cnt.get(key, 0) + 1
                o["sem"], o["signal"] = key, needed[i]
                o["val"] = cnt[key] if needed[i] else None
        for k in cnt:
            self.S(k)
        per_eng = {e: [] for e in self.ENGS}
        for i, o in enumerate(ops):
            per_eng[o["eng"]].append(i)
        S = self.S

        def run_engine(ename, e):
            waited = {}
            for s, v in start_vals.items():
                if v > 0:
                    e.wait_ge(S(s), v)
                    waited[s] = v
            for i in per_eng[ename]:
                o = ops[i]
                need = {}
                for d in o["deps"]:
                    od = ops[d]
                    if od["dma"] is None and o["dma"] is None and od["eng"] == "pe" and ename == "pe":
                        continue
                    s, v = od["sem"], od["val"]
                    if need.get(s, 0) < v:
                        need[s] = v
                for s, v in need.items():
                    if waited.get(s, 0) < v:
                        e.wait_ge(S(s), v)
                        waited[s] = v
                res = o["fn"](e)
                if o["dma"] is not None:
                    lst = res if isinstance(res, (list, tuple)) else [res]
                    assert len(lst) == o["ndma"], (len(lst), o["ndma"])
                    for ins in lst:
                        ins.then_inc(S(o["sem"]), 16)
                elif o["signal"]:
                    res.then_inc(S(o["sem"]), 1)
            if last and ename == "sp":
                for s, v in cnt.items():
                    if waited.get(s, 0) < v:
                        e.wait_ge(S(s), v)

        with self.nc.Block() as block:
            @block.tensor
            def _(e):
                run_engine("pe", e)

            @block.scalar
            def _(e):
                run_engine("act", e)

            @block.vector
            def _(e):
                run_engine("dve", e)

            @block.gpsimd
            def _(e):
                run_engine("pool", e)

            @block.sync
            def _(e):
                run_engine("sp", e)
        self.reset()


def _consts():
    c = {}
    inv = np.power(10000.0, -np.arange(0, 64, 2, dtype=np.float32) / 64.0).astype(np.float32)

    def rope_tab(pos):
        ang = pos.astype(np.float32)[:, None] * inv[None, :]
        ang = np.concatenate([ang, ang], axis=-1)
        cos = np.cos(ang).astype(np.float32)
        sin = np.sin(ang).astype(np.float32)
        sin[:, :32] = -sin[:, :32]
        return cos, sin
    c["cos_p"], c["sin_p"] = rope_tab(np.arange(SEQ))
    cs, ss = rope_tab(np.full((NS,), 8192))
    c["cos_s"], c["sin_s"] = cs, ss
    log_g = np.log1p(-np.power(2.0, -5.0 - np.arange(8, dtype=np.float64)))
    i = np.arange(128, dtype=np.float64)
    c["qsc_p"] = np.exp((i + 1.0)[:, None] * log_g[None, :]).astype(np.float32)
    c["ksc_p"] = (np.exp(-(i + 1.0)[:, None] * log_g[None, :]) * 0.125).astype(np.float32)
    c["qsc_s"] = np.repeat(np.exp(log_g)[None, :], NS, 0).astype(np.float32)
    c["ksc_s"] = np.repeat((np.exp(-log_g) * 0.125)[None, :], NS, 0).astype(np.float32)

    def gtab(power):
        t = np.zeros((128, 4, 64), np.float32)
        for par in range(2):
            for p in range(4):
                t[par * 64:(par + 1) * 64, p, :] = np.exp(power * log_g[2 * p + par])
        return t
    c["g_p"] = gtab(128.0)
    c["g_s"] = gtab(1.0)
    c["g_s2"] = np.repeat(np.exp(log_g)[None, :], 64, 0).astype(np.float32)
    e4 = np.eye(NS, dtype=np.float32)
    c["selS"] = np.repeat(e4[:, :, None], 64, 2).copy()
    c["tsel"] = np.repeat(e4[None, :, :], 128, 0).copy()
    c["esel"] = np.repeat(e4[None, :, :], 8, 0).copy()
    bd = np.zeros((8, 512), np.float32)
    for h in range(8):
        bd[h, h * 64:(h + 1) * 64] = 1.0
    c["bdiag"] = bd
    c["ident"] = np.eye(128, dtype=np.float32)
    jj = np.arange(128)
    c["causT"] = (jj[None, :] >= jj[:, None]).astype(np.float32)
    c["negdiag"] = np.where(jj[None, :] <= jj[:, None], 0.0, CMASK).astype(np.float32)
    c["ones"] = np.ones((128, 128), np.float32)
    oh = np.zeros((4, 4, 128), np.float32)
    for s in range(4):
        oh[s, s, s] = 1.0
    c["ohsel"] = oh.transpose(1, 0, 2).reshape(4, 4 * 128).copy()
    return c


CONST_SHAPES = {
    "cos_p": [SEQ, 64], "sin_p": [SEQ, 64], "cos_s": [NS, 64], "sin_s": [NS, 64],
    "qsc_p": [128, 8], "ksc_p": [128, 8], "qsc_s": [NS, 8], "ksc_s": [NS, 8],
    "g_p": [128, 4, 64], "g_s": [128, 4, 64], "g_s2": [64, 8], "selS": [NS, NS, 64], "tsel": [128, NS, NS], "esel": [8, NS, NS], "bdiag": [8, 512], "ident": [128, 128], "causT": [128, 128],
    "negdiag": [128, 128], "ones": [128, 128], "ohsel": [4, 512],
}


def build_nc():
    nc = bass.Bass("TRN2", target_bir_lowering=False)
    P = Prog(nc)

    def din(name, shape, dt=F32):
        return nc.dram_tensor(name, shape, dt, kind="ExternalInput").ap()

    def dout(name, shape, dt=F32):
        return nc.dram_tensor(name, shape, dt, kind="ExternalOutput").ap()

    I = {}
    for name, shape in [
        ("x_p", [SEQ, D]), ("x_s", [NS, D]), ("cT", [128, 8, 5]), ("ada_w", [D, 6 * D]),
        ("ada_bT", [128, 48]), ("n1T", [128, 8]), ("n2T", [128, 8]), ("w_in", [D, IN_COLS]),
        ("qknw", [1, 128]), ("iknw", [1, 64]), ("gnw", [1, 512]), ("w_out", [D, D]),
        ("router_w", [D, NE]), ("router_b", [1, NE]), ("w_gu", [NE, D, 2 * D]),
        ("bguT", [128, NE, 16]), ("w_dn", [NE, D, D]), ("b_dn", [NE, D]),
        ("state", [NS, 8, 64, 64]),
        ("cache_k", [NPOOL_PAGES * 32, 2048]), ("cache_v", [NPOOL_PAGES * 32, 2048]), ("cache_ik", [NPOOL_PAGES, 8192]),
    ]:
        I[name] = din(name, shape)
    I["ptab"] = din("ptab", [NS, NPAGES], I32)
    I["c16"] = din("c16", [64, 32], I32)
    for name, shape in CONST_SHAPES.items():
        I[name] = din(name, shape)
    O = {}
    for name, shape in [
        ("y_p", [SEQ, D]), ("y_s", [NS, D]), ("k_p", [SEQ, 512]), ("v_p", [SEQ, 512]),
        ("ik_p", [SEQ, 64]), ("ret_p", [8, 64, 64]), ("k_s", [NS, 512]), ("v_s", [NS, 512]),
        ("ik_s", [NS, 64]), ("ret_s", [NS, 8, 64, 64]),
    ]:
        O[name] = dout(name, shape)
    sc_scr = nc.dram_tensor("sc_scr", [NS, 8200], F32, kind="Internal").ap()
    thr_scr = nc.dram_tensor("thr_scr", [1, NS], F32, kind="Internal").ap()
    gs_scr = nc.dram_tensor("gs_scr", [NS, D], F32, kind="Internal").ap()
    x1_scr = nc.dram_tensor("x1_scr", [SEQ + NS, D], F32, kind="Internal").ap()
    h2_scr = nc.dram_tensor("h2_scr", [128, 8, SEQ + NS], BF16, kind="Internal").ap()

    es0 = contextlib.ExitStack()

    def mk(es):
        def T(name, shape, dt=F32):
            return es.enter_context(nc.sbuf_tensor("sb_" + name, shape, dt))
        return T
    T0 = mk(es0)

    def PS(name, shape, dt=F32):
        return es0.enter_context(nc.psum_tensor("ps_" + name, shape, dt))

    PZ = [PS("pz%d" % i, [128, 512]) for i in range(3)]
    PT = PS("pt", [128, 1024], BF16)
    PA = PS("pa", [128, 512])
    PB = PS("pb", [128, 512])
    PO = PS("po", [128, 512])
    PK = PS("pk", [128, 512])

    identf = T0("identf", [128, 128])
    identb = T0("identb", [128, 128], BF16)
    onesf = T0("onesf", [128, 128])
    causT = T0("causT", [128, 128])
    negdiag = T0("negdiag", [128, 128])
    modT = T0("modT", [128, 48, 5])
    A1T = T0("A1T", [128, 8, 5])
    A2T = T0("A2T", [128, 8, 5])
    G1bc = T0("G1bc", [128, D])
    G2bc = T0("G2bc", [128, D])
    cw_all = T0("cw_all", [128, NT + 1, NE])
    eps_t = T0("eps_t", [128, 1])

    def ld(dst, src, key, eng="sp"):
        P.add(eng, lambda e: [e.dma_start(out=dst, in_=src)], w=[key], dma=key)

    ld(identf[:], I["ident"], "identf")
    ld(onesf[:], I["ones"], "onesf")
    ld(causT[:], I["causT"], "causT")
    ld(negdiag[:], I["negdiag"], "negdiag")
    P.add("pool", lambda e: e.tensor_copy(out=identb[:], in_=identf[:]), r=["identf"], w=["identb"])
    P.add("pool", lambda e: e.memset(eps_t[:], EPS), w=["eps"])

    with contextlib.ExitStack() as esA:
        TA = mk(esA)
        cts = TA("cts", [128, 8, 5])
        csil = TA("csil", [128, 8, 5])
        abT = TA("abT", [128, 48])
        n1T = TA("n1T", [128, 8])
        n2T = TA("n2T", [128, 8])
        awb = [TA("awb%d" % i, [128, 8, 512]) for i in range(2)]
        dg = [TA("dg%d" % i, [128, 128]) for i in range(2)]
        ld(cts[:], I["cT"], "cts")
        ld(abT[:], I["ada_bT"], "abT")
        ld(n1T[:], I["n1T"], "n1T")
        ld(n2T[:], I["n2T"], "n2T")
        P.add("act", lambda e: e.activation(out=csil[:], in_=cts[:], func=AF.Silu), r=["cts"], w=["csil"])
        aw_v = I["ada_w"].rearrange("(kc p) n -> p kc n", p=128)
        for blk in range(12):
            b = blk % 2
            ld(awb[b][:], aw_v[:, :, blk * 512:(blk + 1) * 512], "awb%d" % b)
            for j in range(4):
                ch = blk * 4 + j

                def mm(e, b=b, j=j, ch=ch):
                    ins = None
                    for kc in range(8):
                        ins = e.matmul(PA[:, ch * 5:(ch + 1) * 5], lhsT=awb[b][:, kc, j * 128:(j + 1) * 128],
                                       rhs=csil[:, kc, :], start=(kc == 0), stop=(kc == 7))
                    return ins
                P.add("pe", mm, r=["awb%d" % b, "csil"], w=["PA"])
        P.add("dve", lambda e: e.tensor_tensor(out=modT[:], in0=PA[:, 0:240].rearrange("p (c s) -> p c s", s=5),
                                               in1=abT[:].unsqueeze(2).to_broadcast([128, 48, 5]), op=ALU.add),
              r=["PA", "abT"], w=["modT"])
        P.add("dve", lambda e: e.scalar_tensor_tensor(out=A1T[:], in0=modT[:, 8:16, :], scalar=1.0,
                                                      in1=n1T[:].unsqueeze(2).to_broadcast([128, 8, 5]),
                                                      op0=ALU.add, op1=ALU.mult), r=["modT", "n1T"], w=["A1T"])
        P.add("dve", lambda e: e.scalar_tensor_tensor(out=A2T[:], in0=modT[:, 32:40, :], scalar=1.0,
                                                      in1=n2T[:].unsqueeze(2).to_broadcast([128, 8, 5]),
                                                      op0=ALU.add, op1=ALU.mult), r=["modT", "n2T"], w=["A2T"])
        for gi, (vec, Gt, gk) in enumerate([(2, G1bc, "G1bc"), (5, G2bc, "G2bc")]):
            for ch in range(8):
                b = ch % 2
                pz = PZ[ch // 4 % 2 + (gi * 0)]
                P.add("dve", lambda e, b=b, vec=vec, ch=ch: e.tensor_scalar(
                    out=dg[b][:], in0=identf[:], scalar1=modT[:, vec * 8 + ch, 4:5], scalar2=None, op0=ALU.mult),
                    r=["identf", "modT"], w=["dg%d" % b])
                P.add("pe", lambda e, b=b, ch=ch, pz=pz: e.matmul(
                    pz[:, (ch % 4) * 128:(ch % 4 + 1) * 128], lhsT=onesf[:], rhs=dg[b][:], start=True, stop=True),
                    r=["dg%d" % b, "onesf"], w=["PZ%d" % (ch // 4 % 2)])
                if ch % 4 == 3:
                    P.add("act", lambda e, Gt=Gt, ch=ch, pz=pz: e.activation(
                        out=Gt[:, (ch // 4) * 512:(ch // 4 + 1) * 512], in_=pz[:], func=AF.Copy),
                        r=["PZ%d" % (ch // 4 % 2)], w=[gk])
        P.emit()

    esB = contextlib.ExitStack()
    TB = mk(esB)
    w_in = TB("w_in", [128, 8, IN_COLS], BF16)
    w_out = TB("w_out", [128, 8, D], BF16)
    rw = TB("rw", [128, 8, NE], BF16)
    rb_bc = TB("rb_bc", [128, NE])
    qknw = TB("qknw", [128, 128])
    iknw = TB("iknw", [128, 64])
    gnw = TB("gnw", [128, 512])
    w_in_v = I["w_in"].rearrange("(kc p) n -> p kc n", p=128)
    for kc in range(8):
        for (c0, c1) in [(0, 2048), (2048, 4096), (4096, IN_COLS)]:
            P.add("pool", lambda e, kc=kc, c0=c0, c1=c1: [e.dma_start(out=w_in[:, kc, c0:c1], in_=w_in_v[:, kc, c0:c1])],
                  w=["w_in"], dma="w_in")
    w_out_v = I["w_out"].rearrange("(kc p) n -> p kc n", p=128)
    for kc in range(8):
        P.add("pool", lambda e, kc=kc: [e.dma_start(out=w_out[:, kc, :], in_=w_out_v[:, kc, :])], w=["w_out"], dma="w_out")
    P.add("pool", lambda e: [e.dma_start(out=rw[:], in_=I["router_w"].rearrange("(kc p) n -> p kc n", p=128))],
          w=["rw"], dma="rw")
    ld(rb_bc[:], I["router_b"][0:1, :].partition_broadcast(128), "rb_bc")
    ld(qknw[:], I["qknw"][0:1, :].partition_broadcast(128), "qknw")
    ld(iknw[:], I["iknw"][0:1, :].partition_broadcast(128), "iknw")
    ld(gnw[:], I["gnw"][0:1, :].partition_broadcast(128), "gnw")

    xt = TB("xt", [128, D])
    xn = TB("xn", [128, D], BF16)
    hT = TB("hT", [128, 8, 128], BF16)
    ZA = TB("ZA", [128, 1024])
    T1 = TB("T1", [128, 1024])
    T2 = TB("T2", [128, 1024])
    QK = TB("QK", [128, 1024], BF16)
    QKTe = TB("QKTe", [128, 8, 128], BF16)
    QKTo = TB("QKTo", [128, 8, 128], BF16)
    IQ = TB("IQ", [128, 640], BF16)
    IQTe = TB("IQTe", [128, 4, 128], BF16)
    IQTo = TB("IQTo", [128, 4, 128], BF16)
    vr = TB("vr", [128, 512], BF16)
    rgs = TB("rgs", [128, 512])
    PTm = TB("PTm", [128, 8, 128], BF16)
    onrm = TB("onrm", [128, 512])
    mix = TB("mix", [128, D], BF16)
    mixT = PTm
    x1 = xt
    h2Tt = hT
    cosT = TB("cosT", [128, 64])
    sinT = TB("sinT", [128, 64])
    st = TB("st", [128, 64])
    S2 = TB("S2", [128, 4, 64])
    S2b = TB("S2b", [128, 4, 64], BF16)
    qsc = TB("qsc", [128, 8])
    ksc = TB("ksc", [128, 8])
    gtab = TB("gtab", [128, 4, 64])
    wiw = TB("wiw", [128, 8])
    avf = TB("avf", [128, 512])
    ikf = TB("ikf", [128, 64])
    lg = TB("lg", [128, NE])
    lgw = TB("lgw", [128, NE])
    m8 = TB("m8", [128, 8])


    def premix_a(n, x_src, mcol, tabs):
        bc = (n == 128)

        def mview(Tt, lo, hi):
            v = Tt[:, lo:hi, mcol]
            return v.to_broadcast([128, hi - lo, n]) if bc else v
        cos_src, sin_src, qsc_src, ksc_src = tabs
        ld(xt[0:n, :], x_src, "xt")
        ld(cosT[0:n, :], cos_src, "cosT")
        ld(sinT[0:n, :], sin_src, "sinT")
        ld(qsc[0:n, :], qsc_src, "qsc")
        ld(ksc[0:n, :], ksc_src, "ksc")
        P.add("act", lambda e: e.activation(out=xn[0:n, :], in_=xt[0:n, :], func=AF.Square, accum_out=st[0:n, 0:1]),
              r=["xt"], w=["xn", "st0"])
        P.add("act", lambda e: e.activation(out=st[0:n, 1:2], in_=st[0:n, 0:1], func=AF.Sqrt, bias=eps_t[0:n, :], scale=1.0 / D),
              r=["st0", "eps"], w=["st1"])
        P.add("dve", lambda e: e.reciprocal(out=st[0:n, 2:3], in_=st[0:n, 1:2]), r=["st1"], w=["st2"])
        P.add("act", lambda e: e.activation(out=xn[0:n, :], in_=xt[0:n, :], func=AF.Copy, scale=st[0:n, 2:3]),
              r=["xt", "st2"], w=["xn"])
        transpose_mod(n, xn, 8, A1T, 0, mview, hT, "hT")
        inproj(n, 0, 0, 512)
        inproj(n, 1, 512, 1024)
        P.add("act", lambda e: e.activation(out=ZA[0:n, 0:512], in_=PZ[0][0:n, :], func=AF.Copy), r=["PZ0"], w=["ZAa"])
        P.add("act", lambda e: e.activation(out=ZA[0:n, 512:1024], in_=PZ[1][0:n, :], func=AF.Copy), r=["PZ1"], w=["ZAb"])
        inproj(n, 2, 1024, 1536)
        P.add("act", lambda e: e.activation(out=vr[0:n, :], in_=PZ[2][0:n, :], func=AF.Copy), r=["PZ2"], w=["vr"])
        inproj(n, 0, 1536, 2048)
        P.add("act", lambda e: e.activation(out=rgs[0:n, :], in_=PZ[0][0:n, :], func=AF.Silu), r=["PZ0"], w=["rgs"])
        rope(n, ZA[0:n, :], ZA[0:n, :], 16, ["ZAa", "ZAb"], ["ZAa", "ZAb"])
        P.add("dve", lambda e: e.tensor_tensor(out=QK[0:n, 0:512].rearrange("p (h d) -> p h d", d=64),
                                               in0=ZA[0:n, 0:512].rearrange("p (h d) -> p h d", d=64),
                                               in1=qsc[0:n, :].unsqueeze(2).to_broadcast([n, 8, 64]), op=ALU.mult),
              r=["ZAa", "qsc"], w=["QKa"])
        P.add("pool", lambda e: e.tensor_tensor(out=QK[0:n, 512:1024].rearrange("p (h d) -> p h d", d=64),
                                                in0=ZA[0:n, 512:1024].rearrange("p (h d) -> p h d", d=64),
                                                in1=ksc[0:n, :].unsqueeze(2).to_broadcast([n, 8, 64]), op=ALU.mult),
              r=["ZAb", "ksc"], w=["QKb"])

    def transpose_mod(n, src, nblk, AT, boff, mview, dst, dkey):
        def tr(e):
            ins = None
            for kc in range(nblk):
                ins = e.transpose(out=PT[:, kc * 128:kc * 128 + n], in_=src[0:n, kc * 128:(kc + 1) * 128], identity=identb[0:n, 0:n])
            return ins
        P.add("pe", tr, r=["xn", "identb"], w=["PT"])
        PTv = PT[:, :].rearrange("p (k t) -> p k t", t=128)
        tmpT = T1[:, :].rearrange("p (k t) -> p k t", t=128)
        P.add("dve", lambda e: e.tensor_tensor(out=tmpT[:, :, 0:n], in0=PTv[:, :, 0:n], in1=mview(AT, 0, 8), op=ALU.mult),
              r=["PT", "A1T", "A2T"], w=["T1"])
        P.add("pool", lambda e: e.tensor_tensor(out=dst[:, :, 0:n], in0=tmpT[:, :, 0:n], in1=mview(modT, boff, boff + 8), op=ALU.add),
              r=["T1", "modT"], w=[dkey])

    def inproj(n, g, c0, c1):
        pz = PZ[g]

        def f(e):
            ins = None
            for kc in range(8):
                ins = e.matmul(pz[0:n, 0:c1 - c0], lhsT=hT[:, kc, 0:n], rhs=w_in[:, kc, c0:c1], start=(kc == 0), stop=(kc == 7))
            return ins
        P.add("pe", f, r=["hT", "w_in"], w=["PZ%d" % g])

    def rope(n, src, dst, H, rk, wk, eng_a="dve", eng_b="pool"):
        s3 = src.rearrange("p (h d) -> p h d", d=64)
        t1 = T1[0:n, 0:H * 64].rearrange("p (h d) -> p h d", d=64)
        t2 = T2[0:n, 0:H * 64].rearrange("p (h d) -> p h d", d=64)
        d3 = dst.rearrange("p (h d) -> p h d", d=64)
        P.add(eng_a, lambda e: e.tensor_tensor(out=t1, in0=s3, in1=cosT[0:n, :].unsqueeze(1).to_broadcast([n, H, 64]), op=ALU.mult),
              r=rk + ["cosT"], w=["T1"])
        P.add(eng_b, lambda e: e.tensor_tensor(out=t2[:, :, 0:32], in0=s3[:, :, 32:64],
                                               in1=sinT[0:n, 0:32].unsqueeze(1).to_broadcast([n, H, 32]), op=ALU.mult),
              r=rk + ["sinT"], w=["T2a"])
        P.add(eng_b, lambda e: e.tensor_tensor(out=t2[:, :, 32:64], in0=s3[:, :, 0:32],
                                               in1=sinT[0:n, 32:64].unsqueeze(1).to_broadcast([n, H, 32]), op=ALU.mult),
              r=rk + ["sinT"], w=["T2b"])
        P.add(eng_a, lambda e: e.tensor_tensor(out=d3, in0=t1, in1=t2, op=ALU.add), r=["T1", "T2a", "T2b"], w=wk)

    def transposes(n, src, srckeys, nblk):
        def tr(e):
            ins = None
            for b in range(nblk):
                ins = e.transpose(out=PT[:, b * 128:b * 128 + n], in_=src[0:n, b * 128:(b + 1) * 128], identity=identb[0:n, 0:n])
            return ins
        P.add("pe", tr, r=srckeys + ["identb"], w=["PT"])
    PTv = PT[:, :].rearrange("p (k t) -> p k t", t=128)

    def retention_core(n):
        transposes(n, QK, ["QKa", "QKb"], 8)
        P.add("act", lambda e: e.activation(out=QKTe[0:64, :, 0:n], in_=PTv[0:64, :, 0:n], func=AF.Copy), r=["PT"], w=["QKT"])
        P.add("act", lambda e: e.activation(out=QKTo[64:128, :, 0:n], in_=PTv[64:128, :, 0:n], func=AF.Copy), r=["PT"], w=["QKT"])
        for half, pp in ((0, PA), (1, PB)):
            def f(e, half=half, pp=pp):
                ins = None
                for hh in range(4):
                    h = half * 4 + hh
                    p = h // 2
                    Q = QKTe if h % 2 == 0 else QKTo
                    ins = e.matmul(pp[0:n, hh * 128:hh * 128 + n], lhsT=Q[:, 4 + p, 0:n],
                                   rhs=Q[:, p, 0:n], start=True, stop=True)
                return ins
            P.add("pe", f, r=["QKT"], w=["PA" if half == 0 else "PB"])

    def groupnorm_gate(n, po_key, src=None):
        if src is None:
            src = PO[0:n, :]
        pk = po_key if isinstance(po_key, list) else [po_key]
        P.add("act", lambda e: e.activation(out=onrm[0:n, :], in_=src, func=AF.Copy), r=pk, w=["onrm"])
        P.add("act", lambda e: e.activation(out=T1[0:n, 0:512], in_=src, func=AF.Square), r=pk, w=["T1"])
        P.add("dve", lambda e: e.tensor_reduce(out=st[0:n, 8:16], in_=onrm[0:n, :].rearrange("p (h d) -> p h d", d=64), axis=AX.X, op=ALU.add),
              r=["onrm"], w=["st8"])
        P.add("dve", lambda e: e.tensor_reduce(out=st[0:n, 16:24], in_=T1[0:n, 0:512].rearrange("p (h d) -> p h d", d=64), axis=AX.X, op=ALU.add),
              r=["T1"], w=["st16"])
        P.add("dve", lambda e: e.tensor_scalar(out=st[0:n, 8:16], in0=st[0:n, 8:16], scalar1=1.0 / 64, scalar2=None, op0=ALU.mult), r=["st8"], w=["st8"])
        P.add("dve", lambda e: e.tensor_tensor(out=st[0:n, 24:32], in0=st[0:n, 8:16], in1=st[0:n, 8:16], op=ALU.mult), r=["st8"], w=["st24"])
        P.add("dve", lambda e: e.scalar_tensor_tensor(out=st[0:n, 16:24], in0=st[0:n, 16:24], scalar=1.0 / 64, in1=st[0:n, 24:32],
                                                      op0=ALU.mult, op1=ALU.subtract), r=["st16", "st24"], w=["st16"])
        P.add("act", lambda e: e.activation(out=st[0:n, 24:32], in_=st[0:n, 16:24], func=AF.Sqrt, bias=eps_t[0:n, :], scale=1.0),
              r=["st16", "eps"], w=["st24"])
        P.add("dve", lambda e: e.reciprocal(out=st[0:n, 16:24], in_=st[0:n, 24:32]), r=["st24"], w=["st16"])
        on3 = onrm[0:n, :].rearrange("p (h d) -> p h d", d=64)
        P.add("dve", lambda e: e.tensor_tensor(out=on3, in0=on3, in1=st[0:n, 8:16].unsqueeze(2).to_broadcast([n, 8, 64]), op=ALU.subtract),
              r=["onrm", "st8"], w=["onrm"])
        P.add("dve", lambda e: e.tensor_tensor(out=on3, in0=on3, in1=st[0:n, 16:24].unsqueeze(2).to_broadcast([n, 8, 64]), op=ALU.mult),
              r=["onrm", "st16"], w=["onrm"])
        P.add("pool", lambda e: e.tensor_tensor(out=onrm[0:n, :], in0=onrm[0:n, :], in1=gnw[0:n, :], op=ALU.mult), r=["onrm", "gnw"], w=["onrm"])
        P.add("pool", lambda e: e.tensor_tensor(out=mix[0:n, 0:512], in0=onrm[0:n, :], in1=rgs[0:n, :], op=ALU.mult), r=["onrm", "rgs"], w=["mixa"])

    def retention_prompt(t):
        n = 128
        retention_core(n)
        for half, pp in ((0, PA), (1, PB)):
            P.add("dve", lambda e, half=half, pp=pp: e.tensor_tensor(
                out=PTm[:, half * 4:half * 4 + 4, :], in0=pp[:, :].rearrange("p (h t) -> p h t", t=128),
                in1=causT[:, :].unsqueeze(1).to_broadcast([128, 4, 128]), op=ALU.mult),
                r=["PA" if half == 0 else "PB", "causT"], w=["PTm%d" % half])

        def fo(e):
            ins = None
            for h in range(8):
                p, base = h // 2, 64 * (h % 2)
                e.matmul(PO[:, h * 64:(h + 1) * 64], lhsT=PTm[:, h, :], rhs=vr[:, h * 64:(h + 1) * 64], start=True, stop=False)
                Q = QKTe if h % 2 == 0 else QKTo
                ins = e.matmul(PO[:, h * 64:(h + 1) * 64], lhsT=Q[:, p, :], rhs=S2b[:, p, :], start=False, stop=True)
            return ins
        P.add("pe", fo, r=["PTm0", "PTm1", "vr", "QKT", "S2b"], w=["PO"])

        def fkv(e):
            ins = None
            for p in range(4):
                ins = e.matmul(PK[:, p * 128:(p + 1) * 128], lhsT=QK[:, 512 + p * 128:512 + (p + 1) * 128],
                               rhs=vr[:, p * 128:(p + 1) * 128], start=True, stop=True)
            return ins
        P.add("pe", fkv, r=["QKb", "vr"], w=["PK"])
        PKv = PK[:, :].rearrange("p (q c) -> p q c", c=128)
        for par in range(2):
            rs = slice(par * 64, par * 64 + 64)
            P.add("dve", lambda e, rs=rs, par=par: e.tensor_tensor(out=S2[rs, :, :], in0=PKv[rs, :, par * 64:par * 64 + 64], in1=S2[rs, :, :], op=ALU.add),
                  r=["PK", "S2"], w=["S2"])
            P.add("dve", lambda e, rs=rs: e.tensor_tensor(out=S2[rs, :, :], in0=S2[rs, :, :], in1=gtab[rs, :, :], op=ALU.mult),
                  r=["S2", "gtab"], w=["S2"])
        P.add("pool", lambda e: e.tensor_copy(out=S2b[:], in_=S2[:]), r=["S2"], w=["S2b"])
        groupnorm_gate(n, "PO")

    def premix_b(n, t, r0, k_dst, v_dst, ik_dst, prompt):
        inproj(n, 1, 2048, 2560)
        inproj(n, 2, 2560, 3072)
        P.add("act", lambda e: e.activation(out=T1[0:n, 0:512], in_=PZ[1][0:n, :], func=AF.Square), r=["PZ1"], w=["T1"])
        P.add("act", lambda e: e.activation(out=T1[0:n, 512:1024], in_=PZ[2][0:n, :], func=AF.Square), r=["PZ2"], w=["T1"])
        P.add("dve", lambda e: e.tensor_reduce(out=st[0:n, 8:24], in_=T1[0:n, :].rearrange("p (h d) -> p h d", d=64), axis=AX.X, op=ALU.add),
              r=["T1"], w=["st8", "st16"])
        P.add("act", lambda e: e.activation(out=st[0:n, 24:40], in_=st[0:n, 8:24], func=AF.Sqrt, bias=eps_t[0:n, :], scale=1.0 / 64),
              r=["st8", "st16", "eps"], w=["st24", "st32"])
        P.add("dve", lambda e: e.reciprocal(out=st[0:n, 40:56], in_=st[0:n, 24:40]), r=["st24", "st32"], w=["st40"])
        for j, key in ((0, "ZAa"), (1, "ZAb")):
            P.add("dve", lambda e, j=j: e.tensor_tensor(out=ZA[0:n, j * 512:(j + 1) * 512].rearrange("p (h d) -> p h d", d=64),
                                                        in0=PZ[1 + j][0:n, :].rearrange("p (h d) -> p h d", d=64),
                                                        in1=st[0:n, 40 + 8 * j:48 + 8 * j].unsqueeze(2).to_broadcast([n, 8, 64]), op=ALU.mult),
                  r=["PZ%d" % (1 + j), "st40"], w=[key])
            P.add("pool", lambda e, j=j: e.tensor_tensor(out=ZA[0:n, j * 512:(j + 1) * 512].rearrange("p (h d) -> p h d", d=64),
                                                         in0=ZA[0:n, j * 512:(j + 1) * 512].rearrange("p (h d) -> p h d", d=64),
                                                         in1=qknw[0:n, j * 64:(j + 1) * 64].unsqueeze(1).to_broadcast([n, 8, 64]), op=ALU.mult),
                  r=[key, "qknw"], w=[key])
        rope(n, ZA[0:n, :], ZA[0:n, :], 16, ["ZAa", "ZAb"], ["ZAa", "ZAb"])
        P.add("sp", lambda e: [e.dma_start(out=k_dst, in_=ZA[0:n, 512:1024])], r=["ZAb"], dma="kst")
        P.add("act", lambda e: e.activation(out=QK[0:n, :], in_=ZA[0:n, :], func=AF.Copy), r=["ZAa", "ZAb"], w=["QKa", "QKb"])
        if CUTB < 2:
            return
        inproj(n, 0, 3072, 3584)
        P.add("act", lambda e: e.activation(out=avf[0:n, :], in_=PZ[0][0:n, :], func=AF.Copy), r=["PZ0"], w=["avf"])
        P.add("sp", lambda e: [e.dma_start(out=v_dst, in_=avf[0:n, :])], r=["avf"], dma="vst")
        if CUTB < 3:
            return
        inproj(n, 1, 3584, 4096)
        inproj(n, 2, 4096, 4168)
        if CUTB < 4:
            return
        if prompt:
            transposes(n, QK, ["QKa", "QKb"], 8)
            P.add("act", lambda e: e.activation(out=QKTe[0:64, 0:4, 0:n], in_=PTv[0:64, 0:4, 0:n], func=AF.Copy), r=["PT"], w=["QKT"])
            P.add("act", lambda e: e.activation(out=QKTo[64:128, 0:4, 0:n], in_=PTv[64:128, 0:4, 0:n], func=AF.Copy), r=["PT"], w=["QKT"])
            P.add("act", lambda e: e.activation(out=akT[:, :, r0:r0 + n], in_=PTv[:, 4:8, 0:n], func=AF.Copy), r=["PT"], w=["akT"])
            P.add("pool", lambda e: e.tensor_copy(out=Vaug[0:n, t, :, 0:64], in_=avf[0:n, :].rearrange("p (h d) -> p h d", d=64)),
                  r=["avf"], w=["Vaug"])
        if CUTB < 5:
            return
        P.add("act", lambda e: e.activation(out=ZA[0:n, 0:512], in_=PZ[1][0:n, :], func=AF.Copy), r=["PZ1"], w=["ZAa"])
        P.add("act", lambda e: e.activation(out=T1[0:n, 0:64], in_=PZ[2][0:n, 0:64], func=AF.Square, accum_out=st[0:n, 3:4]),
              r=["PZ2"], w=["T1", "st3"])
        P.add("act", lambda e: e.activation(out=st[0:n, 4:5], in_=st[0:n, 3:4], func=AF.Sqrt, bias=eps_t[0:n, :], scale=1.0 / 64),
              r=["st3", "eps"], w=["st4"])
        P.add("dve", lambda e: e.reciprocal(out=st[0:n, 5:6], in_=st[0:n, 4:5]), r=["st4"], w=["st5"])
        P.add("dve", lambda e: e.scalar_tensor_tensor(out=ZA[0:n, 512:576], in0=PZ[2][0:n, 0:64], scalar=st[0:n, 5:6], in1=iknw[0:n, :],
                                                      op0=ALU.mult, op1=ALU.mult), r=["PZ2", "st5", "iknw"], w=["ZAb"])
        P.add("dve", lambda e: e.tensor_scalar(out=wiw[0:n, :], in0=PZ[2][0:n, 64:72], scalar1=(8.0 ** -0.5) * 0.125, scalar2=None, op0=ALU.mult),
              r=["PZ2"], w=["wiw"])
        rope(n, ZA[0:n, 0:576], ZA[0:n, 0:576], 9, ["ZAa", "ZAb"], ["ZAa", "ZAb"])
        P.add("sp", lambda e: [e.dma_start(out=ik_dst, in_=ZA[0:n, 512:576])], r=["ZAb"], dma="ikst")
        if CUTB < 6:
            return
        if prompt:
            P.add("act", lambda e: e.activation(out=IQ[0:n, 0:576], in_=ZA[0:n, 0:576], func=AF.Copy), r=["ZAa", "ZAb"], w=["IQ"])
            P.add("pool", lambda e: e.tensor_copy(out=IQ[0:n, 576:640], in_=ZA[0:n, 512:576]), r=["ZAb"], w=["IQb"])
            transposes(n, IQ, ["IQ", "IQb"], 5)
            P.add("act", lambda e: e.activation(out=IQTe[0:64, :, 0:n], in_=PTv[0:64, 0:4, 0:n], func=AF.Copy), r=["PT"], w=["IQT"])
            P.add("act", lambda e: e.activation(out=IQTo[64:128, :, 0:n], in_=PTv[64:128, 0:4, 0:n], func=AF.Copy), r=["PT"], w=["IQT"])
            P.add("act", lambda e: e.activation(out=ikT2[:, r0:r0 + n], in_=PTv[:, 4, 0:n], func=AF.Copy), r=["PT"], w=["ikT2"])

    def dsa_prompt(t):
        nk = 128 * (t + 1)
        if t >= 2:
            ci = 0
            for c0 in range(0, nk, 512):
                w = min(512, nk - c0)
                for h in range(8):
                    p, base = h // 2, 64 * (h % 2)
                    pp, pk = (PA, "PA") if ci % 2 == 0 else (PB, "PB")
                    ci += 1
                    P.add("pe", lambda e, pp=pp, w=w, c0=c0, p=p, h=h: e.matmul(
                        pp[:, 0:w], lhsT=(IQTe if h % 2 == 0 else IQTo)[:, p, :], rhs=ikT2[:, c0:c0 + w], start=True, stop=True),
                        r=["IQT", "ikT2"], w=[pk])
                    P.add("act", lambda e, pp=pp, w=w: e.activation(out=relu_t[0][:, 0:w], in_=pp[:, 0:w], func=AF.Relu), r=[pk], w=["relu"])
                    if h == 0:
                        P.add("dve", lambda e, w=w, c0=c0, h=h: e.tensor_scalar(out=SC[:, c0:c0 + w], in0=relu_t[0][:, 0:w], scalar1=wiw[:, h:h + 1],
                                                                                scalar2=None, op0=ALU.mult), r=["relu", "wiw"], w=["SC"])
                    else:
                        P.add("dve", lambda e, w=w, c0=c0, h=h: e.scalar_tensor_tensor(out=SC[:, c0:c0 + w], in0=relu_t[0][:, 0:w], scalar=wiw[:, h:h + 1],
                                                                                       in1=SC[:, c0:c0 + w], op0=ALU.mult, op1=ALU.add),
                              r=["relu", "wiw", "SC"], w=["SC"])
            P.add("pool", lambda e: e.tensor_tensor(out=SC[:, nk - 128:nk], in0=SC[:, nk - 128:nk], in1=negdiag[:, :], op=ALU.add),
                  r=["SC", "negdiag"], w=["SC"])
            for rnd in range(32):
                P.add("dve", lambda e: e.max(out=m8[:, :], in_=SC[:, 0:nk]), r=["SC"], w=["m8"])
                P.add("dve", lambda e: e.match_replace(out=SC[:, 0:nk], in_to_replace=m8[:, :], in_values=SC[:, 0:nk], imm_value=REPL),
                      r=["SC", "m8"], w=["SC"])
            P.add("pool", lambda e: e.tensor_scalar(out=MASK[:, 0:nk], in0=SC[:, 0:nk], scalar1=REPL, scalar2=None, op0=ALU.is_equal),
                  r=["SC"], w=["MASK"])
            for k0 in range(0, t + 1, 8):
                kn = min(8, t + 1 - k0)

                def tr(e, k0=k0, kn=kn):
                    ins = None
                    for j in range(kn):
                        ins = e.transpose(out=PT[:, j * 128:(j + 1) * 128], in_=MASK[:, (k0 + j) * 128:(k0 + j + 1) * 128], identity=identb[:, :])
                    return ins
                P.add("pe", tr, r=["MASK", "identb"], w=["PT"])
                P.add("act", lambda e, k0=k0, kn=kn: e.activation(out=MT[:, k0:k0 + kn, :], in_=PTv[:, 0:kn, :], func=AF.Copy), r=["PT"], w=["MT"])
        else:
            if t == 1:
                P.add("pool", lambda e: e.memset(MT[:, 0, :], 1.0), w=["MT"])
            P.add("pool", lambda e: e.tensor_copy(out=MT[:, t, :], in_=causT[:, :]), r=["causT", "MT"], w=["MT"])
        gi = 0
        for h in range(8):
            p, base = h // 2, 64 * (h % 2)
            pout, pokey = (PO, "PO") if h < 4 else (PK, "PK")
            hh = h % 4
            for k0 in range(0, t + 1, 4):
                kn = min(4, t + 1 - k0)
                pp, pk = (PA, "PA") if gi % 2 == 0 else (PB, "PB")
                eb, ek = Eb[gi % 2], "Eb%d" % (gi % 2)
                gi += 1

                def fs(e, pp=pp, k0=k0, kn=kn, p=p, h=h):
                    ins = None
                    for j in range(kn):
                        ins = e.matmul(pp[:, j * 128:(j + 1) * 128], lhsT=akT[:, p, (k0 + j) * 128:(k0 + j + 1) * 128],
                                       rhs=(QKTe if h % 2 == 0 else QKTo)[:, p, :], start=True, stop=True)
                    return ins
                P.add("pe", fs, r=["akT", "QKT"], w=[pk])
                P.add("act", lambda e, pp=pp, eb=eb, kn=kn: e.activation(out=eb[:, 0:kn * 128], in_=pp[:, 0:kn * 128], func=AF.Exp, scale=0.125),
                      r=[pk], w=[ek])
                P.add("pool", lambda e, eb=eb, k0=k0, kn=kn: e.tensor_tensor(
                    out=eb[:, 0:kn * 128].rearrange("p (k q) -> p k q", q=128), in0=eb[:, 0:kn * 128].rearrange("p (k q) -> p k q", q=128),
                    in1=MT[:, k0:k0 + kn, :], op=ALU.mult), r=[ek, "MT"], w=[ek])

                def fv(e, eb=eb, k0=k0, kn=kn, pout=pout, hh=hh, h=h):
                    ins = None
                    for j in range(kn):
                        ins = e.matmul(pout[:, hh * 65:(hh + 1) * 65], lhsT=eb[:, j * 128:(j + 1) * 128], rhs=Vaug[:, k0 + j, h, 0:65],
                                       start=(k0 + j == 0), stop=(k0 + j == t))
                    return ins
                P.add("pe", fv, r=[ek, "Vaug"], w=[pokey])
        for bi, (pout, pokey) in enumerate(((PO, "PO"), (PK, "PK"))):
            pv = pout[:, 0:260].rearrange("p (h c) -> p h c", c=65)
            P.add("dve", lambda e, pv=pv, bi=bi: e.reciprocal(out=st[:, 56 + 4 * bi:60 + 4 * bi], in_=pv[:, :, 64]), r=[pokey], w=["st56_%d" % bi])
            P.add("dve", lambda e, pv=pv, bi=bi: e.tensor_tensor(
                out=mix[:, 512 + bi * 256:512 + (bi + 1) * 256].rearrange("p (h d) -> p h d", d=64), in0=pv[:, :, 0:64],
                in1=st[:, 56 + 4 * bi:60 + 4 * bi].unsqueeze(2).to_broadcast([128, 4, 64]), op=ALU.mult),
                r=[pokey, "st56_%d" % bi], w=["mixb%d" % bi])

    rwf_holder = {}

    def post(n, tile, mcol, Gx1, x1_dst, h2_dst):
        bc = (n == 128)

        def mview(Tt, lo, hi):
            v = Tt[:, lo:hi, mcol]
            return v.to_broadcast([128, hi - lo, n]) if bc else v
        transposes(n, mix, ["mixa", "mixb0", "mixb1"], 8)
        P.add("act", lambda e: e.activation(out=mixT[:, :, 0:n], in_=PTv[:, :, 0:n], func=AF.Copy), r=["PT"], w=["PTm0", "PTm1"])
        for half in range(2):
            pz = PZ[half]

            def f(e, half=half, pz=pz):
                ins = None
                for kc in range(8):
                    ins = e.matmul(pz[0:n, :], lhsT=mixT[:, kc, 0:n], rhs=w_out[:, kc, half * 512:(half + 1) * 512], start=(kc == 0), stop=(kc == 7))
                return ins
            P.add("pe", f, r=["PTm0", "PTm1", "w_out"], w=["PZ%d" % half])
            P.add("dve", lambda e, half=half, pz=pz: e.tensor_tensor(out=T1[0:n, half * 512:(half + 1) * 512], in0=pz[0:n, :],
                                                                    in1=Gx1[0:n, half * 512:(half + 1) * 512], op=ALU.mult),
                  r=["PZ%d" % half, "G1"], w=["T1"])
        P.add("pool", lambda e: e.tensor_tensor(out=xt[0:n, :], in0=xt[0:n, :], in1=T1[0:n, :], op=ALU.add), r=["xt", "T1"], w=["xt"])
        P.add("sp", lambda e: [e.dma_start(out=x1_dst, in_=xt[0:n, :])], r=["xt"], dma="x1st")
        P.add("act", lambda e: e.activation(out=xn[0:n, :], in_=xt[0:n, :], func=AF.Square, accum_out=st[0:n, 0:1]),
              r=["xt"], w=["xn", "st0"])
        P.add("act", lambda e: e.activation(out=st[0:n, 1:2], in_=st[0:n, 0:1], func=AF.Sqrt, bias=eps_t[0:n, :], scale=1.0 / D),
              r=["st0", "eps"], w=["st1"])
        P.add("dve", lambda e: e.reciprocal(out=st[0:n, 2:3], in_=st[0:n, 1:2]), r=["st1"], w=["st2"])
        P.add("act", lambda e: e.activation(out=xn[0:n, :], in_=xt[0:n, :], func=AF.Copy, scale=st[0:n, 2:3]),
              r=["xt", "st2"], w=["xn"])
        transpose_mod(n, xn, 8, A2T, 24, mview, hT, "hT")
        P.add("sp", lambda e: [e.dma_start(out=h2_dst, in_=hT[:, :, 0:n])], r=["hT"], dma="h2st")

        def fr(e):
            ins = None
            for kc in range(8):
                ins = e.matmul(PZ[2][0:n, 0:NE], lhsT=hT[:, kc, 0:n], rhs=rw[:, kc, :], start=(kc == 0), stop=(kc == 7))
            return ins
        if n == 128:
            P.add("pe", fr, r=["hT", "rw"], w=["PZ2"])
        else:
            rwf = rwf_holder["t"]
            P.add("act", lambda e: e.activation(out=T2[0:n, :], in_=xt[0:n, :], func=AF.Copy, scale=st[0:n, 2:3]),
                  r=["xt", "st2"], w=["T2a", "T2b"])

            def trf(e):
                ins = None
                for kc in range(8):
                    ins = e.transpose(out=PA[:, kc * n:(kc + 1) * n], in_=T2[0:n, kc * 128:(kc + 1) * 128], identity=identf[0:n, 0:n])
                return ins
            P.add("pe", trf, r=["T2a", "T2b", "identf"], w=["PA"])
            PAv = PA[:, 0:8 * n].rearrange("p (k t) -> p k t", t=n)
            h2f = T1[:, 512:512 + 8 * n].rearrange("p (k t) -> p k t", t=n)
            P.add("dve", lambda e: e.tensor_tensor(out=h2f, in0=PAv, in1=A2T[:, :, mcol], op=ALU.mult), r=["PA", "A2T"], w=["T1"])
            P.add("dve", lambda e: e.tensor_tensor(out=h2f, in0=h2f, in1=modT[:, 24:32, mcol], op=ALU.add), r=["T1", "modT"], w=["T1"])

            def frf(e):
                ins = None
                for kc in range(8):
                    ins = e.matmul(PZ[2][0:n, 0:NE], lhsT=h2f[:, kc, :], rhs=rwf[:, kc, :], start=(kc == 0), stop=(kc == 7))
                return ins
            P.add("pe", frf, r=["T1", "rwf"], w=["PZ2"])
        P.add("dve", lambda e: e.tensor_tensor(out=lg[0:n, :], in0=PZ[2][0:n, 0:NE], in1=rb_bc[0:n, :], op=ALU.add), r=["PZ2", "rb_bc"], w=["lg"])
        P.add("dve", lambda e: e.max(out=m8[0:n, :], in_=lg[0:n, :]), r=["lg"], w=["m8"])
        P.add("dve", lambda e: e.tensor_scalar(out=st[0:n, 6:7], in0=m8[0:n, 0:1], scalar1=-1.0, scalar2=None, op0=ALU.mult), r=["m8"], w=["st6"])
        P.add("act", lambda e: e.activation(out=lgw[0:n, :], in_=lg[0:n, :], func=AF.Exp, bias=st[0:n, 6:7], scale=1.0), r=["lg", "st6"], w=["lgw"])
        P.add("dve", lambda e: e.tensor_scalar(out=lg[0:n, :], in0=lg[0:n, :], scalar1=m8[0:n, 3:4], scalar2=None, op0=ALU.is_ge), r=["lg", "m8"], w=["lg"])
        P.add("dve", lambda e: e.tensor_tensor(out=lgw[0:n, :], in0=lgw[0:n, :], in1=lg[0:n, :], op=ALU.mult), r=["lgw", "lg"], w=["lgw"])
        P.add("dve", lambda e: e.tensor_reduce(out=st[0:n, 7:8], in_=lgw[0:n, :], axis=AX.X, op=ALU.add), r=["lgw"], w=["st7"])
        P.add("dve", lambda e: e.reciprocal(out=st[0:n, 6:7], in_=st[0:n, 7:8]), r=["st7"], w=["st6"])
        P.add("dve", lambda e: e.tensor_scalar(out=cw_all[0:n, tile, :], in0=lgw[0:n, :], scalar1=st[0:n, 6:7], scalar2=None, op0=ALU.mult),
              r=["lgw", "st6"], w=["cw"])

    esP = contextlib.ExitStack()
    TP = mk(esP)
    akT = TP("akT", [128, 4, SEQ], BF16)
    ikT2 = TP("ikT2", [128, SEQ], BF16)
    Vaug = TP("Vaug", [128, NT, 8, 66], BF16)
    SC = TP("SC", [128, SEQ])
    MASK = TP("MASK", [128, SEQ], BF16)
    MT = TP("MT", [128, NT, 128], BF16)
    Eb = [TP("Eb%d" % i, [128, 512], BF16) for i in range(2)]
    relu_t = [TP("relu%d" % i, [128, 512]) for i in range(1)]
    ld(gtab[:], I["g_p"], "gtab")
    P.add("pool", lambda e: e.memset(Vaug[:], 1.0), w=["Vaug"])
    for zt, zk in ((QKTe, "QKT"), (QKTo, "QKT"), (IQTe, "IQT"), (IQTo, "IQT")):
        P.add("pool", lambda e, zt=zt: e.memset(zt[:], 0.0), w=[zk])
    P.add("pool", lambda e: e.memset(S2[:], 0.0), w=["S2"])
    P.add("pool", lambda e: e.memset(S2b[:], 0.0), w=["S2b"])

    ntiles = int(os.environ.get('NTILES', '0')) or (NT if STAGE >= 2 else 2)
    for t in range(ntiles):
        r0 = t * 128
        premix_a(128, I["x_p"][r0:r0 + 128, :], slice(4, 5),
                 (I["cos_p"][r0:r0 + 128, :], I["sin_p"][r0:r0 + 128, :], I["qsc_p"], I["ksc_p"]))
        if CUT >= 2:
            retention_prompt(t)
        if CUT >= 3:
            premix_b(128, t, r0, O["k_p"][r0:r0 + 128, :], O["v_p"][r0:r0 + 128, :], O["ik_p"][r0:r0 + 128, :], True)
        if CUT >= 4:
            dsa_prompt(t)
        if CUT >= 5:
            post(128, t, slice(4, 5), G1bc, x1_scr[r0:r0 + 128, :], h2_scr[:, :, r0:r0 + 128])
        if STAGE < 5:
            P.add("sp", lambda e, r0=r0: [e.dma_start(out=O["y_p"][r0:r0 + 128, :], in_=xt[:, :])], r=["xt"], dma="x1st")
    if CUT >= 6:
      P.add("sp", lambda e: [e.dma_start(out=O["ret_p"].rearrange("(q par) d v -> (par d) q v", par=2), in_=S2[:, :, :])], r=["S2"], dma="retst")
    P.emit(last=(STAGE < 5))
    esP.close()
    if SAMPLE_ON:
        n = NS
        esS = contextlib.ExitStack()
        TS = mk(esS)
        selb = TS("selb", [NS, NS, 64], BF16)
        self_ = TS("self", [NS, NS, 64])
        tsel = TS("tsel", [128, NS, NS], BF16)
        esel = TS("esel", [8, NS, NS])
        bdiag = TS("bdiag", [8, 512])
        Gs1 = TS("Gs1", [NS, D])
        Gs2 = TS("Gs2", [NS, D])
        Qm = TS("Qm", [128, NS, 2, 4, NS], BF16)
        S2b4 = TS("S2b4", [128, NS, 4, 64], BF16)
        pt_i = TS("pt_i", [128, NS], I32)
        ptf = TS("ptf", [128, NS])
        idxf = TS("idxf", [128, NS, 32])
        c16f = TS("c16f", [64, 32])
        c16 = TS("c16", [64, 32], I32)
        idxc = TS("idxc", [128, NS, 32], I32)
        iqb = TS("iqb", [64, 512])
        wib = TS("wib", [64, 8])
        SCs = TS("SCs", [64, 128])
        MK = TS("MK", [64, 128])
        thrb = TS("thrb", [64, NS])
        ones64 = TS("ones64", [64, 1])
        sc8 = TS("sc8", [64, 4, 8])
        pe8 = TS("pe8", [64, 4, 8])
        nm = TS("nm", [8, 512])
        dd = TS("dd", [8, 8])
        dsb = TS("dsb", [8, 1])
        rwf = TS("rwf", [128, 8, NE])
        rwf_holder["t"] = rwf
        ld(rwf[:], I["router_w"].rearrange("(kc p) n -> p kc n", p=128), "rwf")
        P.add("pool", lambda e: [e.dma_start(out=selb[:], in_=I["selS"])], w=["selb"], dma="selb")
        P.add("pool", lambda e: [e.dma_start(out=tsel[:], in_=I["tsel"])], w=["tsel"], dma="tsel")
        ld(self_[:], I["selS"], "self")
        ld(esel[:], I["esel"], "esel")
        ld(bdiag[:], I["bdiag"], "bdiag")
        ld(c16[:], I["c16"], "c16")
        P.add("pool", lambda e: e.memset(idxf[:], 0.0), w=["idxf"])
        P.add("pool", lambda e: e.memset(ones64[:], 1.0), w=["ones64"])
        for s_ in range(NS):
            P.add("sp", lambda e, s_=s_: [e.dma_start(out=pt_i[0:64, s_:s_ + 1], in_=I["ptab"][s_:s_ + 1, :].rearrange("o j -> j o"), allow_slow_non_contiguous=True)],
                  w=["pt_i%d" % s_], dma="pti")
        PTI = ["pt_i%d" % k for k in range(NS)]
        P.add("dve", lambda e: e.tensor_copy(out=ptf[0:64, :], in_=pt_i[0:64, :]), r=PTI, w=["ptf"])
        P.add("dve", lambda e: e.tensor_copy(out=c16f[:, :], in_=c16[:, :]), r=["c16"], w=["c16f"])
        for s_ in range(NS):
            P.add("dve", lambda e, s_=s_: e.scalar_tensor_tensor(out=idxf[0:64, s_, :], in0=ptf[0:64, s_:s_ + 1].to_broadcast([64, 32]), scalar=32.0,
                                                              in1=c16f[:, :], op0=ALU.mult, op1=ALU.add),
                  r=["ptf", "c16f", "idxf"], w=["idxf%d" % s_])
        P.add("dve", lambda e: e.tensor_copy(out=idxc[:, :, :], in_=idxf[:, :, :]), r=["idxf%d" % k for k in range(NS)], w=["idxc%d" % k for k in range(NS)])
        IDXC = ["idxc%d" % k for k in range(NS)]
        for (vec, Gt, gk) in ((2, Gs1, "G1"), (5, Gs2, "Gs2")):
            for half in range(2):
                def trg(e, vec=vec, half=half):
                    ins = None
                    for j in range(4):
                        ch = half * 4 + j
                        ins = e.transpose(out=PZ[half][0:NS, j * 128:(j + 1) * 128], in_=modT[:, vec * 8 + ch, 0:NS], identity=identf[:, :])
                    return ins
                P.add("pe", trg, r=["modT", "identf"], w=["PZ%d" % half])
                P.add("act", lambda e, Gt=Gt, half=half: e.activation(out=Gt[0:NS, half * 512:(half + 1) * 512], in_=PZ[half][0:NS, :], func=AF.Copy),
                      r=["PZ%d" % half], w=[gk])
        P.add("sp", lambda e: [e.dma_start(out=gs_scr[:, :], in_=Gs2[:, :])], r=["Gs2"], dma="gsst")

        premix_a(n, I["x_s"], slice(0, 4), (I["cos_s"], I["sin_s"], I["qsc_s"], I["ksc_s"]))
        transposes(n, QK, ["QKa", "QKb"], 8)
        P.add("act", lambda e: e.activation(out=QKTe[0:64, :, 0:n], in_=PTv[0:64, :, 0:n], func=AF.Copy), r=["PT"], w=["QKT"])
        P.add("act", lambda e: e.activation(out=QKTo[64:128, :, 0:n], in_=PTv[64:128, :, 0:n], func=AF.Copy), r=["PT"], w=["QKT"])
        for s_ in range(NS):
            for par, Q in enumerate((QKTe, QKTo)):
                P.add("dve", lambda e, s_=s_, par=par, Q=Q: e.tensor_tensor(
                    out=Qm[:, s_, par, :, :], in0=Q[:, 0:4, 0:NS], in1=tsel[:, s_, :].unsqueeze(1).to_broadcast([128, 4, NS]), op=ALU.mult),
                    r=["QKT", "tsel"], w=["Qm%d%d" % (s_, par)])
        ld(m8[0:64, :], I["g_s2"], "m8")
        T1v = T1[0:64, 0:512].rearrange("p (h v) -> p h v", v=64)
        T2v = T2[0:64, 0:512].rearrange("p (h v) -> p h v", v=64)
        for s_ in range(NS):
            P.add("dve", lambda e, s_=s_: e.tensor_scalar(out=mix[0:n, 0:512], in0=QK[0:n, 512:1024], scalar1=identf[0:n, s_:s_ + 1],
                                                          scalar2=None, op0=ALU.mult), r=["QKb", "identf"], w=["mixa"])

            def fkv_s(e):
                ins = None
                for h in range(8):
                    ins = e.matmul(PO[0:64, h * 64:(h + 1) * 64], lhsT=mix[0:n, h * 64:(h + 1) * 64], rhs=vr[0:n, h * 64:(h + 1) * 64],
                                   start=True, stop=True)
                return ins
            P.add("pe", fkv_s, r=["mixa", "vr"], w=["PO"])
            P.add("sp", lambda e, s_=s_: [e.dma_start(out=T1v, in_=I["state"][s_].rearrange("h d v -> d h v"))], w=["T1"], dma="stld")
            P.add("dve", lambda e: e.tensor_tensor(out=T2[0:64, 0:512], in0=PO[0:64, :], in1=T1[0:64, 0:512], op=ALU.add),
                  r=["PO", "T1"], w=["T2a", "T2b"])
            P.add("pool", lambda e: e.tensor_tensor(out=T2v, in0=T2v, in1=m8[0:64, :].unsqueeze(2).to_broadcast([64, 8, 64]), op=ALU.mult),
                  r=["T2a", "T2b", "m8"], w=["T2a", "T2b"])
            P.add("sp", lambda e, s_=s_: [e.dma_start(out=O["ret_s"][s_].rearrange("h d v -> d h v"), in_=T2v)], r=["T2a", "T2b"], dma="retst")
            P.add("sp", lambda e, s_=s_: [e.dma_start(out=S2[:, :, :], in_=I["state"][s_].rearrange("(q par) d v -> (par d) q v", par=2))],
                  w=["S2"], dma="s2ld")
            P.add("pool", lambda e, s_=s_: e.tensor_copy(out=S2b4[:, s_, :, :], in_=S2[:]), r=["S2"], w=["S2b4_%d" % s_])

        def fro(e):
            ins = None
            for h in range(8):
                p, par = h // 2, h % 2
                for s_ in range(NS):
                    ins = e.matmul(PK[0:NS, h * 64:(h + 1) * 64], lhsT=Qm[:, s_, par, p, :], rhs=S2b4[:, s_, p, :],
                                   start=(s_ == 0), stop=(s_ == NS - 1))
            return ins
        P.add("pe", fro, r=["S2b4_%d" % k for k in range(NS)] + ["Qm%d%d" % (k, q) for k in range(NS) for q in range(2)], w=["PK"])
        P.add("dve", lambda e: e.tensor_tensor(out=T1[0:n, 0:512], in0=QK[0:n, 0:512], in1=QK[0:n, 512:1024], op=ALU.mult), r=["QKa", "QKb"], w=["T1"])
        P.add("dve", lambda e: e.tensor_reduce(out=st[0:n, 8:16], in_=T1[0:n, 0:512].rearrange("p (h d) -> p h d", d=64), axis=AX.X, op=ALU.add),
              r=["T1"], w=["st8"])
        P.add("pool", lambda e: e.tensor_tensor(out=T2[0:n, 0:512].rearrange("p (h d) -> p h d", d=64),
                                                in0=vr[0:n, :].rearrange("p (h d) -> p h d", d=64),
                                                in1=st[0:n, 8:16].unsqueeze(2).to_broadcast([n, 8, 64]), op=ALU.mult),
              r=["vr", "st8"], w=["T2a", "T2b"])
        P.add("dve", lambda e: e.tensor_tensor(out=T2[0:n, 0:512], in0=PK[0:n, :], in1=T2[0:n, 0:512], op=ALU.add), r=["PK", "T2a", "T2b"], w=["T2a", "T2b"])
        groupnorm_gate(n, ["T2a", "T2b"], src=T2[0:n, 0:512])
        premix_b(n, None, None, O["k_s"], O["v_s"], O["ik_s"], False)
        P.add("act", lambda e: e.activation(out=IQ[0:n, 0:512], in_=ZA[0:n, 0:512], func=AF.Copy), r=["ZAa"], w=["IQ"])
        P.add("dve", lambda e: e.tensor_tensor(out=T1[0:n, 0:512].rearrange("p (h d) -> p h d", d=64),
                                               in0=IQ[0:n, 0:512].rearrange("p (h d) -> p h d", d=64),
                                               in1=ZA[0:n, 512:576].unsqueeze(1).to_broadcast([n, 8, 64]), op=ALU.mult),
              r=["IQ", "ZAb"], w=["T1"])
        P.add("dve", lambda e: e.tensor_reduce(out=st[0:n, 16:24], in_=T1[0:n, 0:512].rearrange("p (h d) -> p h d", d=64), axis=AX.X, op=ALU.add),
              r=["T1"], w=["st16"])
        P.add("dve", lambda e: e.tensor_scalar(out=st[0:n, 16:24], in0=st[0:n, 16:24], scalar1=0.0, scalar2=None, op0=ALU.max), r=["st16"], w=["st16"])
        P.add("dve", lambda e: e.tensor_tensor(out=st[0:n, 24:32], in0=st[0:n, 16:24], in1=wiw[0:n, :], op=ALU.mult), r=["st16", "wiw"], w=["st24"])
        P.add("dve", lambda e: e.tensor_reduce(out=st[0:n, 32:33], in_=st[0:n, 24:32], axis=AX.X, op=ALU.add), r=["st24"], w=["st32"])
        P.add("sp", lambda e: [e.dma_start(out=sc_scr[0:n, 8192:8193], in_=st[0:n, 32:33], allow_slow_non_contiguous=True)], r=["st32"], w=["scscr"], dma="scst")
        P.emit()
        es1 = contextlib.ExitStack()
        T_1 = mk(es1)
        KI = T_1("KI", [64, 128, 64])
        tmpm = T_1("tmpm", [64, 32, 64])
        red = T_1("red", [64, 32])
        for s_ in range(NS):
            P.add("pool", lambda e, s_=s_: [e.indirect_dma_start(out=KI[:, :, :].rearrange("p r d -> p (r d)"), out_offset=None, in_=I["cache_ik"][:, :],
                                                               in_offset=bass.IndirectOffsetOnAxis(ap=pt_i[0:64, s_:s_ + 1], axis=0))],
                  r=PTI, w=["KI"], dma="KI")
            P.add("pe", lambda e, s_=s_: e.matmul(PA[0:64, 0:512], lhsT=selb[:, s_, :], rhs=IQ[0:n, 0:512], start=True, stop=True), r=["selb", "IQ"], w=["PA"])
            P.add("pe", lambda e, s_=s_: e.matmul(PB[0:64, 0:8], lhsT=self_[:, s_, :], rhs=wiw[0:n, :], start=True, stop=True), r=["self", "wiw"], w=["PB"])
            P.add("act", lambda e: e.activation(out=iqb[:, :], in_=PA[0:64, 0:512], func=AF.Copy), r=["PA"], w=["iqb"])
            P.add("act", lambda e: e.activation(out=wib[:, :], in_=PB[0:64, 0:8], func=AF.Copy), r=["PB"], w=["wib"])
            for half in range(4):
                for h in range(8):
                    P.add("pool", lambda e, half=half, h=h: e.tensor_tensor(
                        out=tmpm[:, :, :], in0=KI[:, half * 32:(half + 1) * 32, :],
                        in1=iqb[:, h * 64:(h + 1) * 64].unsqueeze(1).to_broadcast([64, 32, 64]), op=ALU.mult), r=["KI", "iqb"], w=["tmpm"])
                    P.add("dve", lambda e: e.tensor_reduce(out=red[:, :], in_=tmpm[:, :, :], axis=AX.X, op=ALU.add), r=["tmpm"], w=["red"])
                    P.add("dve", lambda e, h=h: e.tensor_scalar(out=red[:, :], in0=red[:, :], scalar1=0.0, scalar2=wib[:, h:h + 1], op0=ALU.max, op1=ALU.mult),
                          r=["red", "wib"], w=["red"])
                    if h == 0:
                        P.add("dve", lambda e, half=half: e.tensor_copy(out=SCs[:, half * 32:(half + 1) * 32], in_=red[:, :]), r=["red"], w=["SCs"])
                    else:
                        P.add("dve", lambda e, half=half: e.tensor_tensor(out=SCs[:, half * 32:(half + 1) * 32], in0=SCs[:, half * 32:(half + 1) * 32],
                                                                          in1=red[:, :], op=ALU.add), r=["red", "SCs"], w=["SCs"])
            P.add("sp", lambda e, s_=s_: [e.dma_start(out=sc_scr[s_, 0:8192].rearrange("(j r) -> j r", r=128), in_=SCs[:, :])], r=["SCs"], w=["scscr"], dma="scst")
        P.emit()
        es1.close()
        es2 = contextlib.ExitStack()
        T_2 = mk(es2)
        ROW = T_2("ROW", [NS, 8200])
        ld(ROW[:, 0:8193], sc_scr[:, 0:8193], "ROW")
        P.add("dve", lambda e: e.tensor_copy(out=st[0:n, 34:35], in_=ROW[0:n, 8192:8193]), r=["ROW"], w=["st34"])
        for rnd in range(32):
            P.add("dve", lambda e: e.max(out=m8[0:n, :], in_=ROW[0:n, 0:8193]), r=["ROW"], w=["m8"])
            if rnd < 31:
                P.add("dve", lambda e: e.match_replace(out=ROW[0:n, 0:8193], in_to_replace=m8[0:n, :], in_values=ROW[0:n, 0:8193], imm_value=REPL),
                      r=["ROW", "m8"], w=["ROW"])
        P.add("sp", lambda e: [e.dma_start(out=thr_scr.rearrange("o s -> s o"), in_=m8[0:n, 7:8], allow_slow_non_contiguous=True)], r=["m8"], w=["thrscr"], dma="thrst")
        P.add("dve", lambda e: e.tensor_tensor(out=st[0:n, 33:34], in0=st[0:n, 34:35], in1=m8[0:n, 7:8], op=ALU.is_ge), r=["st34", "m8"], w=["st33"])
        P.add("sp", lambda e: [e.dma_start(out=thrb[:, :], in_=thr_scr[0:1, :].partition_broadcast(64))], r=["thrscr"], w=["thrb"], dma="thrb")
        P.emit()
        es2.close()
        es3 = contextlib.ExitStack()
        T_3 = mk(es3)
        Kc = [T_3("Kc%d" % i, [64, 4, 512]) for i in range(1)]
        Vc = [T_3("Vc%d" % i, [64, 4, 512]) for i in range(1)]
        stmp = T_3("stmp", [64, 4, 512])
        aqb = T_3("aqb", [64, 512])
        for s_ in range(NS):
            P.add("sp", lambda e, s_=s_: [e.dma_start(out=SCs[:, :], in_=sc_scr[s_, 0:8192].rearrange("(j r) -> j r", r=128))], r=["scscr"], w=["SCs"], dma="scld")
            P.add("dve", lambda e, s_=s_: e.tensor_scalar(out=MK[:, :], in0=SCs[:, :], scalar1=thrb[:, s_:s_ + 1], scalar2=None, op0=ALU.is_ge),
                  r=["SCs", "thrb"], w=["MK"])
            P.add("pe", lambda e, s_=s_: e.matmul(PA[0:64, 0:512], lhsT=selb[:, s_, :], rhs=QK[0:n, 0:512], start=True, stop=True), r=["selb", "QKa"], w=["PA"])
            P.add("act", lambda e: e.activation(out=aqb[:, :], in_=PA[0:64, 0:512], func=AF.Copy), r=["PA"], w=["aqb"])
            for c in range(32):
                P.add("pool", lambda e, s_=s_, c=c: [e.indirect_dma_start(out=Kc[0][:, :, :].rearrange("p r d -> p (r d)"), out_offset=None, in_=I["cache_k"][:, :],
                                                                       in_offset=bass.IndirectOffsetOnAxis(ap=idxc[0:64, s_, c:c + 1], axis=0))],
                      r=IDXC, w=["Kc"], dma="Kc")
                P.add("pool", lambda e, s_=s_, c=c: [e.indirect_dma_start(out=Vc[0][:, :, :].rearrange("p r d -> p (r d)"), out_offset=None, in_=I["cache_v"][:, :],
                                                                       in_offset=bass.IndirectOffsetOnAxis(ap=idxc[0:64, s_, c:c + 1], axis=0))],
                      r=IDXC, w=["Vc"], dma="Vc")
                P.add("pool", lambda e: e.tensor_tensor(out=stmp[:, :, :], in0=Kc[0][:, :, :], in1=aqb[:, :].unsqueeze(1).to_broadcast([64, 4, 512]), op=ALU.mult),
                      r=["Kc", "aqb"], w=["stmp"])
                P.add("dve", lambda e: e.tensor_reduce(out=sc8[:, :, :].rearrange("p r h -> p (r h)"), in_=stmp[:, :, :].rearrange("p r (h d) -> p (r h) d", d=64),
                                                       axis=AX.X, op=ALU.add), r=["stmp"], w=["sc8"])
                P.add("act", lambda e: e.activation(out=pe8[:, :, :], in_=sc8[:, :, :], func=AF.Exp, scale=0.125), r=["sc8"], w=["pe8"])
                P.add("dve", lambda e, c=c: e.tensor_tensor(out=pe8[:, :, :], in0=pe8[:, :, :], in1=MK[:, c * 4:(c + 1) * 4].unsqueeze(2).to_broadcast([64, 4, 8]), op=ALU.mult),
                      r=["pe8", "MK"], w=["pe8"])

                def fav(e, c=c):
                    ins = None
                    for r_ in range(4):
                        e.matmul(PO[0:8, 0:512], lhsT=pe8[:, r_, :], rhs=Vc[0][:, r_, :], start=(c == 0 and r_ == 0), stop=(c == 31 and r_ == 3))
                        ins = e.matmul(PB[0:8, 0:1], lhsT=pe8[:, r_, :], rhs=ones64[:, :], start=(c == 0 and r_ == 0), stop=(c == 31 and r_ == 3))
                    return ins
                P.add("pe", fav, r=["pe8", "Vc", "ones64"], w=["PO", "PB"])
            P.add("dve", lambda e: e.tensor_tensor(out=nm[:, :], in0=PO[0:8, :], in1=bdiag[:, :], op=ALU.mult), r=["PO", "bdiag"], w=["nm"])
            P.add("act", lambda e: e.activation(out=dsb[:, :], in_=PB[0:8, 0:1], func=AF.Copy), r=["PB"], w=["dsb"])
            P.add("dve", lambda e: e.tensor_scalar(out=dd[:, :], in0=identf[0:8, 0:8], scalar1=dsb[:, 0:1], scalar2=None, op0=ALU.mult), r=["dsb", "identf"], w=["dd"])
            P.add("pe", lambda e, s_=s_: e.matmul(PZ[0][0:NS, 0:512], lhsT=esel[:, s_, :], rhs=nm[:, :], start=(s_ == 0), stop=(s_ == NS - 1)),
                  r=["esel", "nm"], w=["PZ0"])
            P.add("pe", lambda e, s_=s_: e.matmul(PZ[1][0:NS, 0:8], lhsT=esel[:, s_, :], rhs=dd[:, :], start=(s_ == 0), stop=(s_ == NS - 1)),
                  r=["esel", "dd"], w=["PZ1"])
        P.add("dve", lambda e: e.tensor_tensor(out=T1[0:n, 0:512], in0=QK[0:n, 0:512], in1=QK[0:n, 512:1024], op=ALU.mult), r=["QKa", "QKb"], w=["T1"])
        P.add("dve", lambda e: e.tensor_reduce(out=st[0:n, 40:48], in_=T1[0:n, 0:512].rearrange("p (h d) -> p h d", d=64), axis=AX.X, op=ALU.add),
              r=["T1"], w=["st40"])
        P.add("act", lambda e: e.activation(out=st[0:n, 48:56], in_=st[0:n, 40:48], func=AF.Exp, scale=0.125), r=["st40"], w=["st48"])
        P.add("dve", lambda e: e.tensor_scalar(out=st[0:n, 48:56], in0=st[0:n, 48:56], scalar1=st[0:n, 33:34], scalar2=None, op0=ALU.mult),
              r=["st48", "st33"], w=["st48"])
        P.add("pool", lambda e: e.tensor_tensor(out=T2[0:n, 0:512].rearrange("p (h d) -> p h d", d=64),
                                                in0=avf[0:n, :].rearrange("p (h d) -> p h d", d=64),
                                                in1=st[0:n, 48:56].unsqueeze(2).to_broadcast([n, 8, 64]), op=ALU.mult),
              r=["avf", "st48"], w=["T2a", "T2b"])
        P.add("dve", lambda e: e.tensor_tensor(out=T2[0:n, 0:512], in0=PZ[0][0:n, :], in1=T2[0:n, 0:512], op=ALU.add), r=["PZ0", "T2a", "T2b"], w=["T2a", "T2b"])
        P.add("dve", lambda e: e.tensor_tensor(out=st[0:n, 56:64], in0=PZ[1][0:n, 0:8], in1=st[0:n, 48:56], op=ALU.add), r=["PZ1", "st48"], w=["st56"])
        P.add("dve", lambda e: e.reciprocal(out=st[0:n, 40:48], in_=st[0:n, 56:64]), r=["st56"], w=["st40"])
        P.add("dve", lambda e: e.tensor_tensor(out=mix[0:n, 512:1024].rearrange("p (h d) -> p h d", d=64),
                                               in0=T2[0:n, 0:512].rearrange("p (h d) -> p h d", d=64),
                                               in1=st[0:n, 40:48].unsqueeze(2).to_broadcast([n, 8, 64]), op=ALU.mult),
              r=["T2a", "T2b", "st40"], w=["mixb0", "mixb1"])
        post(n, NT, slice(0, 4), Gs1, x1_scr[SEQ:SEQ + n, :], h2_scr[:, :, SEQ:SEQ + n])
        P.emit()
        es3.close()
        esS.close()

    esB.close()
    if STAGE >= 5:
        moe_phase(nc, P, mk, I, O, ntiles, h2_scr, x1_scr, cw_all, G2bc, identf,
                  PZ, PA, PB, PO, PK, SAMPLE_ON, gs_scr)
    es0.close()
    return nc


def moe_phase(nc, P, mk, I, O, ntiles, h2_scr, x1_scr, cw_all, G2bc, identf, PZ, PA, PB, PO, PK, sample_on, gs_scr):
    esC = contextlib.ExitStack()
    TC = mk(esC)
    NTOK = SEQ + NS
    ntok_p = ntiles * 128
    tiles = [(t, t * 128, 128) for t in range(ntiles)]
    blocks = []
    c = 0
    while c < ntok_p:
        w = min(512, ntok_p - c)
        blocks.append((c, w))
        c += w
    if sample_on:
        tiles.append((NT, SEQ, NS))
        blocks.append((SEQ, NS))
    h2T = TC("h2T", [128, 8, NTOK], BF16)
    acc = TC("acc", [128, NT + 1, D])
    actT = TC("actT", [128, 8, NTOK], BF16)
    bgu = TC("bgu", [128, NE, 16])
    bdn = TC("bdn", [NE, D])
    esW = contextlib.ExitStack()
    TW = mk(esW)
    gu = TW("gu", [128, 8, 2 * D], BF16)
    dn = TW("dn", [128, 8, D], BF16)
    tmp3 = TW("tmp3", [128, 3, 512])
    gt = [tmp3[:, 0, :]] * 2
    gs = [tmp3[:, 1, :]] * 2
    ut = [tmp3[:, 2, :]] * 2

    def ld(dst, src, key, eng="sp"):
        P.add(eng, lambda e: [e.dma_start(out=dst, in_=src)], w=[key], dma=key)
    ld(h2T[:, :, 0:ntok_p], h2_scr[:, :, 0:ntok_p], "h2T")
    if sample_on:
        ld(h2T[:, :, SEQ:NTOK], h2_scr[:, :, SEQ:NTOK], "h2T")
    ld(bgu[:], I["bguT"], "bgu")
    ld(bdn[:], I["b_dn"], "bdn")
    P.add("dve", lambda en: en.tensor_scalar(out=cw_all[:, 0:ntiles, :], in0=cw_all[:, 0:ntiles, :], scalar1=1.0 / 1.702, scalar2=None, op0=ALU.mult), r=["cw"], w=["cw"])
    if sample_on:
        P.add("dve", lambda en: en.tensor_scalar(out=cw_all[0:NS, NT, :], in0=cw_all[0:NS, NT, :], scalar1=1.0 / 1.702, scalar2=None, op0=ALU.mult), r=["cw"], w=["cw"])
    P.add("dve", lambda en: en.tensor_scalar(out=bdn[:], in0=bdn[:], scalar1=1.702, scalar2=None, op0=ALU.mult), r=["bdn"], w=["bdn"])
    gu_v = I["w_gu"].rearrange("e (kc p) n -> e p kc n", p=128)
    dn_v = I["w_dn"].rearrange("e (kc p) n -> e p kc n", p=128)
    GUK = ["gu%d" % k for k in range(8)]
    DNK = ["dn%d" % k for k in range(8)]

    def load_gu(e):
        for kc in range(8):
            P.add("pool", lambda en, e=e, kc=kc: [en.dma_start(out=gu[:, kc, :], in_=gu_v[e, :, kc, :])], w=[GUK[kc]], dma=GUK[kc])

    def load_dn(e):
        for kc in range(8):
            P.add("pool", lambda en, e=e, kc=kc: [en.dma_start(out=dn[:, kc, :], in_=dn_v[e, :, kc, :])], w=[DNK[kc]], dma=DNK[kc])

    pairs = [(PZ[0], "PZ0", PZ[1], "PZ1"), (PZ[2], "PZ2", PA, "PA"), (PB, "PB", PO, "PO")]
    ui = 0
    load_gu(0)
    load_dn(0)
    for e in range(NEXP):
        for (c0, w) in blocks:
            for j in range(8):
                pg, pgk, pu, puk = pairs[ui % 3]
                b = 0
                ui += 1
                for (pp, ppk, col) in ((pg, pgk, j * 128), (pu, puk, D + j * 128)):
                    def mm(en, pp=pp, col=col, c0=c0, w=w):
                        ins = None
                        for kc in range(8):
                            ins = en.matmul(pp[:, 0:w], lhsT=gu[:, kc, col:col + 128], rhs=h2T[:, kc, c0:c0 + w],
                                            start=(kc == 0), stop=(kc == 7))
                        return ins
                    P.add("pe", mm, r=GUK + ["h2T"], w=[ppk])
                P.add("dve", lambda en, pg=pg, b=b, w=w, e=e, j=j: en.tensor_scalar(
                    out=gt[b][:, 0:w], in0=pg[:, 0:w], scalar1=bgu[:, e, j:j + 1], scalar2=7.0, op0=ALU.add, op1=ALU.min),
                    r=[pgk, "bgu"], w=["gt%d" % b])
                P.add("act", lambda en, b=b, w=w: en.activation(out=gs[b][:, 0:w], in_=gt[b][:, 0:w], func=AF.Silu, scale=1.702),
                      r=["gt%d" % b], w=["gs%d" % b])
                P.add("dve", lambda en, pu=pu, b=b, w=w, e=e, j=j: en.tensor_scalar(
                    out=ut[b][:, 0:w], in0=pu[:, 0:w], scalar1=bgu[:, e, 8 + j:9 + j], scalar2=7.0, op0=ALU.add, op1=ALU.min),
                    r=[puk, "bgu"], w=["ut%d" % b])
                P.add("dve", lambda en, b=b, w=w: en.tensor_scalar(
                    out=ut[b][:, 0:w], in0=ut[b][:, 0:w], scalar1=-7.0, scalar2=1.0, op0=ALU.max, op1=ALU.add),
                    r=["ut%d" % b], w=["ut%d" % b])
                P.add("pool", lambda en, b=b, w=w, j=j, c0=c0: en.tensor_tensor(
                    out=actT[:, j, c0:c0 + w], in0=ut[b][:, 0:w], in1=gs[b][:, 0:w], op=ALU.mult),
                    r=["ut%d" % b, "gs%d" % b], w=["actT"])
        if e + 1 < NEXP:
            load_gu(e + 1)
        for (t, tok0, n) in tiles:
            p0, p0k, p1, p1k = pairs[ui % 3]
            ui += 1
            for half, (pd, pdk) in enumerate(((p0, p0k), (p1, p1k))):
                def md(en, pd=pd, half=half, tok0=tok0, n=n):
                    ins = None
                    for fc in range(8):
                        ins = en.matmul(pd[0:n, :], lhsT=actT[:, fc, tok0:tok0 + n], rhs=dn[:, fc, half * 512:(half + 1) * 512],
                                        start=(fc == 0), stop=(fc == 7))
                    return ins
                P.add("pe", md, r=DNK + ["actT"], w=[pdk])
                av = acc[0:n, t, half * 512:(half + 1) * 512]
                if e == 0:
                    P.add("dve", lambda en, pd=pd, av=av, n=n, t=t, e=e: en.tensor_scalar(
                        out=av, in0=pd[0:n, :], scalar1=cw_all[0:n, t, e:e + 1], scalar2=None, op0=ALU.mult),
                        r=[pdk, "cw"], w=["acc%d_%d" % (t, half)])
                else:
                    P.add("dve", lambda en, pd=pd, av=av, n=n, t=t, e=e: en.scalar_tensor_tensor(
                        out=av, in0=pd[0:n, :], scalar=cw_all[0:n, t, e:e + 1], in1=av, op0=ALU.mult, op1=ALU.add),
                        r=[pdk, "cw", "acc%d_%d" % (t, half)], w=["acc%d_%d" % (t, half)])
        if e + 1 < NEXP:
            load_dn(e + 1)
    P.emit()
    esW.close()
    xo = TC("xo", [128, D])
    yo = TC("yo", [128, D])
    cwT = TC("cwT", [NE, 128])
    gs2 = TC("gs2", [NS, D])
    if sample_on:
        P.add("sp", lambda en: [en.dma_start(out=gs2[:, :], in_=gs_scr[:, :])], w=["G2bc"], dma="gs2")
    for (t, tok0, n) in tiles:
        P.add("pe", lambda en, n=n, t=t: en.transpose(out=PK[0:NE, 0:n], in_=cw_all[0:n, t, :], identity=identf[0:n, 0:n]),
              r=["cw", "identf"], w=["PK"])
        P.add("act", lambda en, n=n: en.activation(out=cwT[:, 0:n], in_=PK[0:NE, 0:n], func=AF.Copy), r=["PK"], w=["cwT"])
        P.add("sp", lambda en, n=n, tok0=tok0: [en.dma_start(out=xo[0:n, :], in_=x1_scr[tok0:tok0 + n, :])], w=["xo"], dma="xo")
        for half, (pp, ppk) in enumerate(((PA, "PA"), (PB, "PB"))):
            P.add("pe", lambda en, pp=pp, half=half, n=n: en.matmul(pp[0:n, :], lhsT=cwT[:, 0:n], rhs=bdn[:, half * 512:(half + 1) * 512],
                                                                   start=True, stop=True), r=["cwT", "bdn"], w=[ppk])
            P.add("dve", lambda en, pp=pp, half=half, n=n, t=t: en.tensor_tensor(
                out=yo[0:n, half * 512:(half + 1) * 512], in0=pp[0:n, :], in1=acc[0:n, t, half * 512:(half + 1) * 512], op=ALU.add),
                r=[ppk, "acc%d_%d" % (t, half)], w=["yo%d" % half])
        Gt = G2bc if t < NT else gs2
        P.add("pool", lambda en, n=n, Gt=Gt: en.tensor_tensor(out=yo[0:n, :], in0=yo[0:n, :], in1=Gt[0:n, :], op=ALU.mult),
              r=["yo0", "yo1", "G2bc"], w=["yo0", "yo1"])
        P.add("dve", lambda en, n=n: en.tensor_tensor(out=yo[0:n, :], in0=yo[0:n, :], in1=xo[0:n, :], op=ALU.add),
              r=["yo0", "yo1", "xo"], w=["yo0", "yo1"])
        dst = O["y_p"][tok0:tok0 + n, :] if t < NT else O["y_s"][0:n, :]
        P.add("sp", lambda en, n=n, dst=dst: [en.dma_start(out=dst, in_=yo[0:n, :])], r=["yo0", "yo1"], dma="yst")
    P.emit(last=True)
    esC.close()


_NC_CACHE = {}


def _get_nc():
    if "nc" not in _NC_CACHE:
        _NC_CACHE["nc"] = build_nc()
    return _NC_CACHE["nc"]


def kernel(x_prompt, x_sample, cache_k, cache_v, cache_idx_k, state_ret, page_table,
           c_prompt, c_sample, ada_w, ada_b, norm1_w, w_in, q_norm_w, k_norm_w,
           idx_k_norm_w, ret_gn_w, w_out, norm2_w, router_w, router_b,
           w_gate_up, b_gate_up, w_down, b_down):
    f = lambda a: np.ascontiguousarray(np.asarray(a))
    consts = _consts()
    shared = {
        "ada_w": f(ada_w[0]),
        "ada_bT": f(np.asarray(ada_b[0]).reshape(48, 128).T),
        "n1T": f(np.asarray(norm1_w[0]).reshape(8, 128).T),
        "n2T": f(np.asarray(norm2_w[0]).reshape(8, 128).T),
        "w_in": f(w_in[0]),
        "qknw": f(np.concatenate([np.asarray(q_norm_w[0]), np.asarray(k_norm_w[0])])[None, :]),
        "iknw": f(np.asarray(idx_k_norm_w[0])[None, :]),
        "gnw": f(np.asarray(ret_gn_w[0])[None, :]),
        "w_out": f(w_out[0]),
        "router_w": f(router_w[0]),
        "router_b": f(np.asarray(router_b[0])[None, :]),
        "w_gu": f(w_gate_up[0]),
        "bguT": f(np.asarray(b_gate_up[0]).reshape(NE, 16, 128).transpose(2, 0, 1)),
        "w_dn": f(w_down[0]),
        "b_dn": f(b_down[0]),
    }
    shared["cache_k"] = f(np.asarray(cache_k[0]).reshape(NPOOL_PAGES * 32, 2048))
    shared["cache_v"] = f(np.asarray(cache_v[0]).reshape(NPOOL_PAGES * 32, 2048))
    shared["cache_ik"] = f(np.asarray(cache_idx_k[0]).reshape(NPOOL_PAGES, 8192))
    shared["c16"] = np.ascontiguousarray(np.repeat(np.arange(32, dtype=np.int32)[None, :], 64, 0))
    shared.update(consts)
    in_maps = []
    ncores = 1 if DBG else NCORES
    for c in range(ncores):
        s0 = c * NS
        call = np.concatenate([np.asarray(c_sample[s0:s0 + NS]), np.asarray(c_prompt[c:c + 1])], 0)
        m = dict(shared)
        m["x_p"] = f(x_prompt[c])
        m["x_s"] = f(np.asarray(x_sample[s0:s0 + NS, 0]))
        m["cT"] = f(call.T.reshape(8, 128, 5).transpose(1, 0, 2))
        m["state"] = f(state_ret[0, s0:s0 + NS])
        m["ptab"] = f(np.asarray(page_table[s0:s0 + NS]).astype(np.int32))
        in_maps.append(m)
    nc = _get_nc()
    res = run_bass_kernel_spmd(nc, in_maps, core_ids=list(range(ncores)))
    R = res.results
    cat = lambda k: np.stack([np.asarray(R[min(c, ncores - 1)][k]) for c in range(NCORES)], 0)
    y_p = cat("y_p")
    y_s = cat("y_s").reshape(32, 1, D)
    k_p = cat("k_p").reshape(1, 8, SEQ, 8, 64)
    v_p = cat("v_p").reshape(1, 8, SEQ, 8, 64)
    ik_p = cat("ik_p").reshape(1, 8, SEQ, 64)
    ret_p = cat("ret_p").reshape(1, 8, 8, 64, 64)
    k_s = cat("k_s").reshape(1, 32, 1, 8, 64)
    v_s = cat("v_s").reshape(1, 32, 1, 8, 64)
    ik_s = cat("ik_s").reshape(1, 32, 1, 64)
    ret_s = cat("ret_s").reshape(1, 32, 8, 64, 64)
    return (y_p, y_s, k_p, v_p, ik_p, ret_p, k_s, v_s, ik_s, ret_s)
```

```python
import contextlib
import math
import os
import numpy as np
import concourse.bass as bass
import concourse.mybir as mybir
from concourse.bass_utils import run_bass_kernel_spmd

F32 = mybir.dt.float32
BF16 = mybir.dt.bfloat16
I32 = mybir.dt.int32
AF = mybir.ActivationFunctionType
ALU = mybir.AluOpType
AX = mybir.AxisListType

NCORES = 8
D = 1024
SEQ = 2048
NT = SEQ // 128
NS = 4
NPAGES = 64
DBG = int(os.environ.get("MKDBG", "0"))
NPOOL_PAGES = 2560
IN_COLS = 4168
EPS = 1e-6
BIG = 1.0e30
REPL = -1.0e30
CMASK = -3.0e30
NE = 32
CUT = 99
NEXP = int(os.environ.get("NEXP", "32"))
SAMPLE_ON = int(os.environ.get("SAMPLE_ON", "1"))
CUTB = int(os.environ.get("CUTB", "99"))
STAGE = 99


class Prog:
    ENGS = ("pe", "act", "dve", "pool", "sp")

    def __init__(self, nc):
        self.nc = nc
        self.sem = {}
        self.cnt = {}
        self.reset()

    def reset(self):
        self.ops = []
        self.last_w = {}
        self.readers = {}

    def S(self, key):
        if key not in self.sem:
            self.sem[key] = self.nc.alloc_semaphore(name="s%d" % len(self.sem))
        return self.sem[key]

    def add(self, eng, fn, r=(), w=(), dma=None, ndma=1):
        idx = len(self.ops)
        deps = set()
        for k in r:
            if k in self.last_w:
                deps.add(self.last_w[k])
        for k in w:
            if k in self.last_w:
                deps.add(self.last_w[k])
            for x in self.readers.get(k, ()):
                deps.add(x)
        for k in r:
            self.readers.setdefault(k, []).append(idx)
        for k in w:
            self.last_w[k] = idx
            self.readers[k] = []
        deps.discard(idx)
        self.ops.append(dict(eng=eng, fn=fn, deps=deps, dma=dma, ndma=ndma))
        return idx

    def emit(self, last=False):
        ops = self.ops
        start_vals = dict(self.cnt)
        needed = [False] * len(ops)
        for o in ops:
            for d in o["deps"]:
                od = ops[d]
                if od["dma"] is None and o["dma"] is None and od["eng"] == "pe" and o["eng"] == "pe":
                    continue
                needed[d] = True
        cnt = self.cnt
        for i, o in enumerate(ops):
            if o["dma"] is not None:
                key = "dma:" + str(o["dma"])
                cnt[key] = cnt.get(key, 0) + 16 * o["ndma"]
                o["sem"], o["val"], o["signal"] = key, cnt[key], True
            else:
                key = "eng:" + o["eng"]
                if needed[i]:
                    cnt[key] = cnt.get(key, 0) + 1
                o["sem"], o["signal"] = key, needed[i]
                o["val"] = cnt[key] if needed[i] else None
        for k in cnt:
            self.S(k)
        per_eng = {e: [] for e in self.ENGS}
        for i, o in enumerate(ops):
            per_eng[o["eng"]].append(i)
        S = self.S

        def run_engine(ename, e):
            waited = {}
            for s, v in start_vals.items():
                if v > 0:
                    e.wait_ge(S(s), v)
                    waited[s] = v
            for i in per_eng[ename]:
                o = ops[i]
                need = {}
                for d in o["deps"]:
                    od = ops[d]
                    if od["dma"] is None and o["dma"] is None and od["eng"] == "pe" and ename == "pe":
                        continue
                    s, v = od["sem"], od["val"]
                    if need.get(s, 0) < v:
                        need[s] = v
                for s, v in need.items():
                    if waited.get(s, 0) < v:
                        e.wait_ge(S(s), v)
                        waited[s] = v
                res = o["fn"](e)
                if o["dma"] is not None:
                    lst = res if isinstance(res, (list, tuple)) else [res]
                    assert len(lst) == o["ndma"], (len(lst), o["ndma"])
                    for ins in lst:
                        ins.then_inc(S(o["sem"]), 16)
                elif o["signal"]:
                    res.then_inc(S(o["sem"]), 1)
            if last and ename == "sp":
                for s, v in cnt.items():
                    if waited.get(s, 0) < v:
                        e.wait_ge(S(s), v)

        with self.nc.Block() as block:
            @block.tensor
            def _(e):
                run_engine("pe", e)

            @block.scalar
            def _(e):
                run_engine("act", e)

            @block.vector
            def _(e):
                run_engine("dve", e)

            @block.gpsimd
            def _(e):
                run_engine("pool", e)

            @block.sync
            def _(e):
                run_engine("sp", e)
        self.reset()


def _consts():
    c = {}
    inv = np.power(10000.0, -np.arange(0, 64, 2, dtype=np.float32) / 64.0).astype(np.float32)

    def rope_tab(pos):
        ang = pos.astype(np.float32)[:, None] * inv[None, :]
        ang = np.concatenate([ang, ang], axis=-1)
        cos = np.cos(ang).astype(np.float32)
        sin = np.sin(ang).astype(np.float32)
        sin[:, :32] = -sin[:, :32]
        return cos, sin
    c["cos_p"], c["sin_p"] = rope_tab(np.arange(SEQ))
    cs, ss = rope_tab(np.full((NS,), 8192))
    c["cos_s"], c["sin_s"] = cs, ss
    log_g = np.log1p(-np.power(2.0, -5.0 - np.arange(8, dtype=np.float64)))
    i = np.arange(128, dtype=np.float64)
    c["qsc_p"] = np.exp((i + 1.0)[:, None] * log_g[None, :]).astype(np.float32)
    c["ksc_p"] = (np.exp(-(i + 1.0)[:, None] * log_g[None, :]) * 0.125).astype(np.float32)
    c["qsc_s"] = np.repeat(np.exp(log_g)[None, :], NS, 0).astype(np.float32)
    c["ksc_s"] = np.repeat((np.exp(-log_g) * 0.125)[None, :], NS, 0).astype(np.float32)

    def gtab(power):
        t = np.zeros((128, 4, 64), np.float32)
        for par in range(2):
            for p in range(4):
                t[par * 64:(par + 1) * 64, p, :] = np.exp(power * log_g[2 * p + par])
        return t
    c["g_p"] = gtab(128.0)
    c["g_s"] = gtab(1.0)
    c["g_s2"] = np.repeat(np.exp(log_g)[None, :], 64, 0).astype(np.float32)
    e4 = np.eye(NS, dtype=np.float32)
    c["selS"] = np.repeat(e4[:, :, None], 64, 2).copy()
    c["tsel"] = np.repeat(e4[None, :, :], 128, 0).copy()
    c["esel"] = np.repeat(e4[None, :, :], 8, 0).copy()
    bd = np.zeros((8, 512), np.float32)
    for h in range(8):
        bd[h, h * 64:(h + 1) * 64] = 1.0
    c["bdiag"] = bd
    c["ident"] = np.eye(128, dtype=np.float32)
    jj = np.arange(128)
    c["causT"] = (jj[None, :] >= jj[:, None]).astype(np.float32)
    c["negdiag"] = np.where(jj[None, :] <= jj[:, None], 0.0, CMASK).astype(np.float32)
    c["ones"] = np.ones((128, 128), np.float32)
    oh = np.zeros((4, 4, 128), np.float32)
    for s in range(4):
        oh[s, s, s] = 1.0
    c["ohsel"] = oh.transpose(1, 0, 2).reshape(4, 4 * 128).copy()
    return c


CONST_SHAPES = {
    "cos_p": [SEQ, 64], "sin_p": [SEQ, 64], "cos_s": [NS, 64], "sin_s": [NS, 64],
    "qsc_p": [128, 8], "ksc_p": [128, 8], "qsc_s": [NS, 8], "ksc_s": [NS, 8],
    "g_p": [128, 4, 64], "g_s": [128, 4, 64], "g_s2": [64, 8], "selS": [NS, NS, 64], "tsel": [128, NS, NS], "esel": [8, NS, NS], "bdiag": [8, 512], "ident": [128, 128], "causT": [128, 128],
    "negdiag": [128, 128], "ones": [128, 128], "ohsel": [4, 512],
}


def build_nc():
    nc = bass.Bass("TRN2", target_bir_lowering=False)
    P = Prog(nc)

    def din(name, shape, dt=F32):
        return nc.dram_tensor(name, shape, dt, kind="ExternalInput").ap()

    def dout(name, shape, dt=F32):
        return nc.dram_tensor(name, shape, dt, kind="ExternalOutput").ap()

    I = {}
    for name, shape in [
        ("x_p", [SEQ, D]), ("x_s", [NS, D]), ("cT", [128, 8, 5]), ("ada_w", [D, 6 * D]),
        ("ada_bT", [128, 48]), ("n1T", [128, 8]), ("n2T", [128, 8]), ("w_in", [D, IN_COLS]),
        ("qknw", [1, 128]), ("iknw", [1, 64]), ("gnw", [1, 512]), ("w_out", [D, D]),
        ("router_w", [D, NE]), ("router_b", [1, NE]), ("w_gu", [NE, D, 2 * D]),
        ("bguT", [128, NE, 16]), ("w_dn", [NE, D, D]), ("b_dn", [NE, D]),
        ("state", [NS, 8, 64, 64]),
        ("cache_k", [NPOOL_PAGES * 32, 2048]), ("cache_v", [NPOOL_PAGES * 32, 2048]), ("cache_ik", [NPOOL_PAGES, 8192]),
    ]:
        I[name] = din(name, shape)
    I["ptab"] = din("ptab", [NS, NPAGES], I32)
    I["c16"] = din("c16", [64, 32], I32)
    for name, shape in CONST_SHAPES.items():
        I[name] = din(name, shape)
    O = {}
    for name, shape in [
        ("y_p", [SEQ, D]), ("y_s", [NS, D]), ("k_p", [SEQ, 512]), ("v_p", [SEQ, 512]),
        ("ik_p", [SEQ, 64]), ("ret_p", [8, 64, 64]), ("k_s", [NS, 512]), ("v_s", [NS, 512]),
        ("ik_s", [NS, 64]), ("ret_s", [NS, 8, 64, 64]),
    ]:
        O[name] = dout(name, shape)
    sc_scr = nc.dram_tensor("sc_scr", [NS, 8200], F32, kind="Internal").ap()
    thr_scr = nc.dram_tensor("thr_scr", [1, NS], F32, kind="Internal").ap()
    gs_scr = nc.dram_tensor("gs_scr", [NS, D], F32, kind="Internal").ap()
    x1_scr = nc.dram_tensor("x1_scr", [SEQ + NS, D], F32, kind="Internal").ap()
    h2_scr = nc.dram_tensor("h2_scr", [128, 8, SEQ + NS], BF16, kind="Internal").ap()

    es0 = contextlib.ExitStack()

    def mk(es):
        def T(name, shape, dt=F32):
            return es.enter_context(nc.sbuf_tensor("sb_" + name, shape, dt))
        return T
    T0 = mk(es0)

    def PS(name, shape, dt=F32):
        return es0.enter_context(nc.psum_tensor("ps_" + name, shape, dt))

    PZ = [PS("pz%d" % i, [128, 512]) for i in range(3)]
    PT = PS("pt", [128, 1024], BF16)
    PA = PS("pa", [128, 512])
    PB = PS("pb", [128, 512])
    PO = PS("po", [128, 512])
    PK = PS("pk", [128, 512])

    identf = T0("identf", [128, 128])
    identb = T0("identb", [128, 128], BF16)
    onesf = T0("onesf", [128, 128])
    causT = T0("causT", [128, 128])
    negdiag = T0("negdiag", [128, 128])
    modT = T0("modT", [128, 48, 5])
    A1T = T0("A1T", [128, 8, 5])
    A2T = T0("A2T", [128, 8, 5])
    G1bc = T0("G1bc", [128, D])
    G2bc = T0("G2bc", [128, D])
    cw_all = T0("cw_all", [128, NT + 1, NE])
    eps_t = T0("eps_t", [128, 1])

    def ld(dst, src, key, eng="sp"):
        P.add(eng, lambda e: [e.dma_start(out=dst, in_=src)], w=[key], dma=key)

    ld(identf[:], I["ident"], "identf")
    ld(onesf[:], I["ones"], "onesf")
    ld(causT[:], I["causT"], "causT")
    ld(negdiag[:], I["negdiag"], "negdiag")
    P.add("pool", lambda e: e.tensor_copy(out=identb[:], in_=identf[:]), r=["identf"], w=["identb"])
    P.add("pool", lambda e: e.memset(eps_t[:], EPS), w=["eps"])

    with contextlib.ExitStack() as esA:
        TA = mk(esA)
        cts = TA("cts", [128, 8, 5])
        csil = TA("csil", [128, 8, 5])
        abT = TA("abT", [128, 48])
        n1T = TA("n1T", [128, 8])
        n2T = TA("n2T", [128, 8])
        awb = [TA("awb%d" % i, [128, 8, 512]) for i in range(2)]
        dg = [TA("dg%d" % i, [128, 128]) for i in range(2)]
        ld(cts[:], I["cT"], "cts")
        ld(abT[:], I["ada_bT"], "abT")
        ld(n1T[:], I["n1T"], "n1T")
        ld(n2T[:], I["n2T"], "n2T")
        P.add("act", lambda e: e.activation(out=csil[:], in_=cts[:], func=AF.Silu), r=["cts"], w=["csil"])
        aw_v = I["ada_w"].rearrange("(kc p) n -> p kc n", p=128)
        for blk in range(12):
            b = blk % 2
            ld(awb[b][:], aw_v[:, :, blk * 512:(blk + 1) * 512], "awb%d" % b)
            for j in range(4):
                ch = blk * 4 + j

                def mm(e, b=b, j=j, ch=ch):
                    ins = None
                    for kc in range(8):
                        ins = e.matmul(PA[:, ch * 5:(ch + 1) * 5], lhsT=awb[b][:, kc, j * 128:(j + 1) * 128],
                                       rhs=csil[:, kc, :], start=(kc == 0), stop=(kc == 7))
                    return ins
                P.add("pe", mm, r=["awb%d" % b, "csil"], w=["PA"])
        P.add("dve", lambda e: e.tensor_tensor(out=modT[:], in0=PA[:, 0:240].rearrange("p (c s) -> p c s", s=5),
                                               in1=abT[:].unsqueeze(2).to_broadcast([128, 48, 5]), op=ALU.add),
              r=["PA", "abT"], w=["modT"])
        P.add("dve", lambda e: e.scalar_tensor_tensor(out=A1T[:], in0=modT[:, 8:16, :], scalar=1.0,
                                                      in1=n1T[:].unsqueeze(2).to_broadcast([128, 8, 5]),
                                                      op0=ALU.add, op1=ALU.mult), r=["modT", "n1T"], w=["A1T"])
        P.add("dve", lambda e: e.scalar_tensor_tensor(out=A2T[:], in0=modT[:, 32:40, :], scalar=1.0,
                                                      in1=n2T[:].unsqueeze(2).to_broadcast([128, 8, 5]),
                                                      op0=ALU.add, op1=ALU.mult), r=["modT", "n2T"], w=["A2T"])
        for gi, (vec, Gt, gk) in enumerate([(2, G1bc, "G1bc"), (5, G2bc, "G2bc")]):
            for ch in range(8):
                b = ch % 2
                pz = PZ[ch // 4 % 2 + (gi * 0)]
                P.add("dve", lambda e, b=b, vec=vec, ch=ch: e.tensor_scalar(
                    out=dg[b][:], in0=identf[:], scalar1=modT[:, vec * 8 + ch, 4:5], scalar2=None, op0=ALU.mult),
                    r=["identf", "modT"], w=["dg%d" % b])
                P.add("pe", lambda e, b=b, ch=ch, pz=pz: e.matmul(
                    pz[:, (ch % 4) * 128:(ch % 4 + 1) * 128], lhsT=onesf[:], rhs=dg[b][:], start=True, stop=True),
                    r=["dg%d" % b, "onesf"], w=["PZ%d" % (ch // 4 % 2)])
                if ch % 4 == 3:
                    P.add("act", lambda e, Gt=Gt, ch=ch, pz=pz: e.activation(
                        out=Gt[:, (ch // 4) * 512:(ch // 4 + 1) * 512], in_=pz[:], func=AF.Copy),
                        r=["PZ%d" % (ch // 4 % 2)], w=[gk])
        P.emit()

    esB = contextlib.ExitStack()
    TB = mk(esB)
    w_in = TB("w_in", [128, 8, IN_COLS], BF16)
    w_out = TB("w_out", [128, 8, D], BF16)
    rw = TB("rw", [128, 8, NE], BF16)
    rb_bc = TB("rb_bc", [128, NE])
    qknw = TB("qknw", [128, 128])
    iknw = TB("iknw", [128, 64])
    gnw = TB("gnw", [128, 512])
    w_in_v = I["w_in"].rearrange("(kc p) n -> p kc n", p=128)
    for kc in range(8):
        for (c0, c1) in [(0, 2048), (2048, 4096), (4096, IN_COLS)]:
            P.add("pool", lambda e, kc=kc, c0=c0, c1=c1: [e.dma_start(out=w_in[:, kc, c0:c1], in_=w_in_v[:, kc, c0:c1])],
                  w=["w_in"], dma="w_in")
    w_out_v = I["w_out"].rearrange("(kc p) n -> p kc n", p=128)
    for kc in range(8):
        P.add("pool", lambda e, kc=kc: [e.dma_start(out=w_out[:, kc, :], in_=w_out_v[:, kc, :])], w=["w_out"], dma="w_out")
    P.add("pool", lambda e: [e.dma_start(out=rw[:], in_=I["router_w"].rearrange("(kc p) n -> p kc n", p=128))],
          w=["rw"], dma="rw")
    ld(rb_bc[:], I["router_b"][0:1, :].partition_broadcast(128), "rb_bc")
    ld(qknw[:], I["qknw"][0:1, :].partition_broadcast(128), "qknw")
    ld(iknw[:], I["iknw"][0:1, :].partition_broadcast(128), "iknw")
    ld(gnw[:], I["gnw"][0:1, :].partition_broadcast(128), "gnw")

    xt = TB("xt", [128, D])
    xn = TB("xn", [128, D], BF16)
    hT = TB("hT", [128, 8, 128], BF16)
    ZA = TB("ZA", [128, 1024])
    T1 = TB("T1", [128, 1024])
    T2 = TB("T2", [128, 1024])
    QK = TB("QK", [128, 1024], BF16)
    QKTe = TB("QKTe", [128, 8, 128], BF16)
    QKTo = TB("QKTo", [128, 8, 128], BF16)
    IQ = TB("IQ", [128, 640], BF16)
    IQTe = TB("IQTe", [128, 4, 128], BF16)
    IQTo = TB("IQTo", [128, 4, 128], BF16)
    vr = TB("vr", [128, 512], BF16)
    rgs = TB("rgs", [128, 512])
    PTm = TB("PTm", [128, 8, 128], BF16)
    onrm = TB("onrm", [128, 512])
    mix = TB("mix", [128, D], BF16)
    mixT = PTm
    x1 = xt
    h2Tt = hT
    cosT = TB("cosT", [128, 64])
    sinT = TB("sinT", [128, 64])
    st = TB("st", [128, 64])
    S2 = TB("S2", [128, 4, 64])
    S2b = TB("S2b", [128, 4, 64], BF16)
    qsc = TB("qsc", [128, 8])
    ksc = TB("ksc", [128, 8])
    gtab = TB("gtab", [128, 4, 64])
    wiw = TB("wiw", [128, 8])
    avf = TB("avf", [128, 512])
    ikf = TB("ikf", [128, 64])
    lg = TB("lg", [128, NE])
    lgw = TB("lgw", [128, NE])
    m8 = TB("m8", [128, 8])


    def premix_a(n, x_src, mcol, tabs):
        bc = (n == 128)

        def mview(Tt, lo, hi):
            v = Tt[:, lo:hi, mcol]
            return v.to_broadcast([128, hi - lo, n]) if bc else v
        cos_src, sin_src, qsc_src, ksc_src = tabs
        ld(xt[0:n, :], x_src, "xt")
        ld(cosT[0:n, :], cos_src, "cosT")
        ld(sinT[0:n, :], sin_src, "sinT")
        ld(qsc[0:n, :], qsc_src, "qsc")
        ld(ksc[0:n, :], ksc_src, "ksc")
        P.add("act", lambda e: e.activation(out=xn[0:n, :], in_=xt[0:n, :], func=AF.Square, accum_out=st[0:n, 0:1]),
              r=["xt"], w=["xn", "st0"])
        P.add("act", lambda e: e.activation(out=st[0:n, 1:2], in_=st[0:n, 0:1], func=AF.Sqrt, bias=eps_t[0:n, :], scale=1.0 / D),
              r=["st0", "eps"], w=["st1"])
        P.add("dve", lambda e: e.reciprocal(out=st[0:n, 2:3], in_=st[0:n, 1:2]), r=["st1"], w=["st2"])
        P.add("act", lambda e: e.activation(out=xn[0:n, :], in_=xt[0:n, :], func=AF.Copy, scale=st[0:n, 2:3]),
              r=["xt", "st2"], w=["xn"])
        transpose_mod(n, xn, 8, A1T, 0, mview, hT, "hT")
        inproj(n, 0, 0, 512)
        inproj(n, 1, 512, 1024)
        P.add("act", lambda e: e.activation(out=ZA[0:n, 0:512], in_=PZ[0][0:n, :], func=AF.Copy), r=["PZ0"], w=["ZAa"])
        P.add("act", lambda e: e.activation(out=ZA[0:n, 512:1024], in_=PZ[1][0:n, :], func=AF.Copy), r=["PZ1"], w=["ZAb"])
        inproj(n, 2, 1024, 1536)
        P.add("act", lambda e: e.activation(out=vr[0:n, :], in_=PZ[2][0:n, :], func=AF.Copy), r=["PZ2"], w=["vr"])
        inproj(n, 0, 1536, 2048)
        P.add("act", lambda e: e.activation(out=rgs[0:n, :], in_=PZ[0][0:n, :], func=AF.Silu), r=["PZ0"], w=["rgs"])
        rope(n, ZA[0:n, :], ZA[0:n, :], 16, ["ZAa", "ZAb"], ["ZAa", "ZAb"])
        P.add("dve", lambda e: e.tensor_tensor(out=QK[0:n, 0:512].rearrange("p (h d) -> p h d", d=64),
                                               in0=ZA[0:n, 0:512].rearrange("p (h d) -> p h d", d=64),
                                               in1=qsc[0:n, :].unsqueeze(2).to_broadcast([n, 8, 64]), op=ALU.mult),
              r=["ZAa", "qsc"], w=["QKa"])
        P.add("pool", lambda e: e.tensor_tensor(out=QK[0:n, 512:1024].rearrange("p (h d) -> p h d", d=64),
                                                in0=ZA[0:n, 512:1024].rearrange("p (h d) -> p h d", d=64),
                                                in1=ksc[0:n, :].unsqueeze(2).to_broadcast([n, 8, 64]), op=ALU.mult),
              r=["ZAb", "ksc"], w=["QKb"])

    def transpose_mod(n, src, nblk, AT, boff, mview, dst, dkey):
        def tr(e):
            ins = None
            for kc in range(nblk):
                ins = e.transpose(out=PT[:, kc * 128:kc * 128 + n], in_=src[0:n, kc * 128:(kc + 1) * 128], identity=identb[0:n, 0:n])
            return ins
        P.add("pe", tr, r=["xn", "identb"], w=["PT"])
        PTv = PT[:, :].rearrange("p (k t) -> p k t", t=128)
        tmpT = T1[:, :].rearrange("p (k t) -> p k t", t=128)
        P.add("dve", lambda e: e.tensor_tensor(out=tmpT[:, :, 0:n], in0=PTv[:, :, 0:n], in1=mview(AT, 0, 8), op=ALU.mult),
              r=["PT", "A1T", "A2T"], w=["T1"])
        P.add("pool", lambda e: e.tensor_tensor(out=dst[:, :, 0:n], in0=tmpT[:, :, 0:n], in1=mview(modT, boff, boff + 8), op=ALU.add),
              r=["T1", "modT"], w=[dkey])

    def inproj(n, g, c0, c1):
        pz = PZ[g]

        def f(e):
            ins = None
            for kc in range(8):
                ins = e.matmul(pz[0:n, 0:c1 - c0], lhsT=hT[:, kc, 0:n], rhs=w_in[:, kc, c0:c1], start=(kc == 0), stop=(kc == 7))
            return ins
        P.add("pe", f, r=["hT", "w_in"], w=["PZ%d" % g])

    def rope(n, src, dst, H, rk, wk, eng_a="dve", eng_b="pool"):
        s3 = src.rearrange("p (h d) -> p h d", d=64)
        t1 = T1[0:n, 0:H * 64].rearrange("p (h d) -> p h d", d=64)
        t2 = T2[0:n, 0:H * 64].rearrange("p (h d) -> p h d", d=64)
        d3 = dst.rearrange("p (h d) -> p h d", d=64)
        P.add(eng_a, lambda e: e.tensor_tensor(out=t1, in0=s3, in1=cosT[0:n, :].unsqueeze(1).to_broadcast([n, H, 64]), op=ALU.mult),
              r=rk + ["cosT"], w=["T1"])
        P.add(eng_b, lambda e: e.tensor_tensor(out=t2[:, :, 0:32], in0=s3[:, :, 32:64],
                                               in1=sinT[0:n, 0:32].unsqueeze(1).to_broadcast([n, H, 32]), op=ALU.mult),
              r=rk + ["sinT"], w=["T2a"])
        P.add(eng_b, lambda e: e.tensor_tensor(out=t2[:, :, 32:64], in0=s3[:, :, 0:32],
                                               in1=sinT[0:n, 32:64].unsqueeze(1).to_broadcast([n, H, 32]), op=ALU.mult),
              r=rk + ["sinT"], w=["T2b"])
        P.add(eng_a, lambda e: e.tensor_tensor(out=d3, in0=t1, in1=t2, op=ALU.add), r=["T1", "T2a", "T2b"], w=wk)

    def transposes(n, src, srckeys, nblk):
        def tr(e):
            ins = None
            for b in range(nblk):
                ins = e.transpose(out=PT[:, b * 128:b * 128 + n], in_=src[0:n, b * 128:(b + 1) * 128], identity=identb[0:n, 0:n])
            return ins
        P.add("pe", tr, r=srckeys + ["identb"], w=["PT"])
    PTv = PT[:, :].rearrange("p (k t) -> p k t", t=128)

    def retention_core(n):
        transposes(n, QK, ["QKa", "QKb"], 8)
        P.add("act", lambda e: e.activation(out=QKTe[0:64, :, 0:n], in_=PTv[0:64, :, 0:n], func=AF.Copy), r=["PT"], w=["QKT"])
        P.add("act", lambda e: e.activation(out=QKTo[64:128, :, 0:n], in_=PTv[64:128, :, 0:n], func=AF.Copy), r=["PT"], w=["QKT"])
        for half, pp in ((0, PA), (1, PB)):
            def f(e, half=half, pp=pp):
                ins = None
                for hh in range(4):
                    h = half * 4 + hh
                    p = h // 2
                    Q = QKTe if h % 2 == 0 else QKTo
                    ins = e.matmul(pp[0:n, hh * 128:hh * 128 + n], lhsT=Q[:, 4 + p, 0:n],
                                   rhs=Q[:, p, 0:n], start=True, stop=True)
                return ins
            P.add("pe", f, r=["QKT"], w=["PA" if half == 0 else "PB"])

    def groupnorm_gate(n, po_key, src=None):
        if src is None:
            src = PO[0:n, :]
        pk = po_key if isinstance(po_key, list) else [po_key]
        P.add("act", lambda e: e.activation(out=onrm[0:n, :], in_=src, func=AF.Copy), r=pk, w=["onrm"])
        P.add("act", lambda e: e.activation(out=T1[0:n, 0:512], in_=src, func=AF.Square), r=pk, w=["T1"])
        P.add("dve", lambda e: e.tensor_reduce(out=st[0:n, 8:16], in_=onrm[0:n, :].rearrange("p (h d) -> p h d", d=64), axis=AX.X, op=ALU.add),
              r=["onrm"], w=["st8"])
        P.add("dve", lambda e: e.tensor_reduce(out=st[0:n, 16:24], in_=T1[0:n, 0:512].rearrange("p (h d) -> p h d", d=64), axis=AX.X, op=ALU.add),
              r=["T1"], w=["st16"])
        P.add("dve", lambda e: e.tensor_scalar(out=st[0:n, 8:16], in0=st[0:n, 8:16], scalar1=1.0 / 64, scalar2=None, op0=ALU.mult), r=["st8"], w=["st8"])
        P.add("dve", lambda e: e.tensor_tensor(out=st[0:n, 24:32], in0=st[0:n, 8:16], in1=st[0:n, 8:16], op=ALU.mult), r=["st8"], w=["st24"])
        P.add("dve", lambda e: e.scalar_tensor_tensor(out=st[0:n, 16:24], in0=st[0:n, 16:24], scalar=1.0 / 64, in1=st[0:n, 24:32],
                                                      op0=ALU.mult, op1=ALU.subtract), r=["st16", "st24"], w=["st16"])
        P.add("act", lambda e: e.activation(out=st[0:n, 24:32], in_=st[0:n, 16:24], func=AF.Sqrt, bias=eps_t[0:n, :], scale=1.0),
              r=["st16", "eps"], w=["st24"])
        P.add("dve", lambda e: e.reciprocal(out=st[0:n, 16:24], in_=st[0:n, 24:32]), r=["st24"], w=["st16"])
        on3 = onrm[0:n, :].rearrange("p (h d) -> p h d", d=64)
        P.add("dve", lambda e: e.tensor_tensor(out=on3, in0=on3, in1=st[0:n, 8:16].unsqueeze(2).to_broadcast([n, 8, 64]), op=ALU.subtract),
              r=["onrm", "st8"], w=["onrm"])
        P.add("dve", lambda e: e.tensor_tensor(out=on3, in0=on3, in1=st[0:n, 16:24].unsqueeze(2).to_broadcast([n, 8, 64]), op=ALU.mult),
              r=["onrm", "st16"], w=["onrm"])
        P.add("pool", lambda e: e.tensor_tensor(out=onrm[0:n, :], in0=onrm[0:n, :], in1=gnw[0:n, :], op=ALU.mult), r=["onrm", "gnw"], w=["onrm"])
        P.add("pool", lambda e: e.tensor_tensor(out=mix[0:n, 0:512], in0=onrm[0:n, :], in1=rgs[0:n, :], op=ALU.mult), r=["onrm", "rgs"], w=["mixa"])

    def retention_prompt(t):
        n = 128
        retention_core(n)
        for half, pp in ((0, PA), (1, PB)):
            P.add("dve", lambda e, half=half, pp=pp: e.tensor_tensor(
                out=PTm[:, half * 4:half * 4 + 4, :], in0=pp[:, :].rearrange("p (h t) -> p h t", t=128),
                in1=causT[:, :].unsqueeze(1).to_broadcast([128, 4, 128]), op=ALU.mult),
                r=["PA" if half == 0 else "PB", "causT"], w=["PTm%d" % half])

        def fo(e):
            ins = None
            for h in range(8):
                p, base = h // 2, 64 * (h % 2)
                e.matmul(PO[:, h * 64:(h + 1) * 64], lhsT=PTm[:, h, :], rhs=vr[:, h * 64:(h + 1) * 64], start=True, stop=False)
                Q = QKTe if h % 2 == 0 else QKTo
                ins = e.matmul(PO[:, h * 64:(h + 1) * 64], lhsT=Q[:, p, :], rhs=S2b[:, p, :], start=False, stop=True)
            return ins
        P.add("pe", fo, r=["PTm0", "PTm1", "vr", "QKT", "S2b"], w=["PO"])

        def fkv(e):
            ins = None
            for p in range(4):
                ins = e.matmul(PK[:, p * 128:(p + 1) * 128], lhsT=QK[:, 512 + p * 128:512 + (p + 1) * 128],
                               rhs=vr[:, p * 128:(p + 1) * 128], start=True, stop=True)
            return ins
        P.add("pe", fkv, r=["QKb", "vr"], w=["PK"])
        PKv = PK[:, :].rearrange("p (q c) -> p q c", c=128)
        for par in range(2):
            rs = slice(par * 64, par * 64 + 64)
            P.add("dve", lambda e, rs=rs, par=par: e.tensor_tensor(out=S2[rs, :, :], in0=PKv[rs, :, par * 64:par * 64 + 64], in1=S2[rs, :, :], op=ALU.add),
                  r=["PK", "S2"], w=["S2"])
            P.add("dve", lambda e, rs=rs: e.tensor_tensor(out=S2[rs, :, :], in0=S2[rs, :, :], in1=gtab[rs, :, :], op=ALU.mult),
                  r=["S2", "gtab"], w=["S2"])
        P.add("pool", lambda e: e.tensor_copy(out=S2b[:], in_=S2[:]), r=["S2"], w=["S2b"])
        groupnorm_gate(n, "PO")

    def premix_b(n, t, r0, k_dst, v_dst, ik_dst, prompt):
        inproj(n, 1, 2048, 2560)
        inproj(n, 2, 2560, 3072)
        P.add("act", lambda e: e.activation(out=T1[0:n, 0:512], in_=PZ[1][0:n, :], func=AF.Square), r=["PZ1"], w=["T1"])
        P.add("act", lambda e: e.activation(out=T1[0:n, 512:1024], in_=PZ[2][0:n, :], func=AF.Square), r=["PZ2"], w=["T1"])
        P.add("dve", lambda e: e.tensor_reduce(out=st[0:n, 8:24], in_=T1[0:n, :].rearrange("p (h d) -> p h d", d=64), axis=AX.X, op=ALU.add),
              r=["T1"], w=["st8", "st16"])
        P.add("act", lambda e: e.activation(out=st[0:n, 24:40], in_=st[0:n, 8:24], func=AF.Sqrt, bias=eps_t[0:n, :], scale=1.0 / 64),
              r=["st8", "st16", "eps"], w=["st24", "st32"])
        P.add("dve", lambda e: e.reciprocal(out=st[0:n, 40:56], in_=st[0:n, 24:40]), r=["st24", "st32"], w=["st40"])
        for j, key in ((0, "ZAa"), (1, "ZAb")):
            P.add("dve", lambda e, j=j: e.tensor_tensor(out=ZA[0:n, j * 512:(j + 1) * 512].rearrange("p (h d) -> p h d", d=64),
                                                        in0=PZ[1 + j][0:n, :].rearrange("p (h d) -> p h d", d=64),
                                                        in1=st[0:n, 40 + 8 * j:48 + 8 * j].unsqueeze(2).to_broadcast([n, 8, 64]), op=ALU.mult),
                  r=["PZ%d" % (1 + j), "st40"], w=[key])
            P.add("pool", lambda e, j=j: e.tensor_tensor(out=ZA[0:n, j * 512:(j + 1) * 512].rearrange("p (h d) -> p h d", d=64),
                                                         in0=ZA[0:n, j * 512:(j + 1) * 512].rearrange("p (h d) -> p h d", d=64),
                                                         in1=qknw[0:n, j * 64:(j + 1) * 64].unsqueeze(1).to_broadcast([n, 8, 64]), op=ALU.mult),
                  r=[key, "qknw"], w=[key])
        rope(n, ZA[0:n, :], ZA[0:n, :], 16, ["ZAa", "ZAb"], ["ZAa", "ZAb"])
        P.add("sp", lambda e: [e.dma_start(out=k_dst, in_=ZA[0:n, 512:1024])], r=["ZAb"], dma="kst")
        P.add("act", lambda e: e.activation(out=QK[0:n, :], in_=ZA[0:n, :], func=AF.Copy), r=["ZAa", "ZAb"], w=["QKa", "QKb"])
        if CUTB < 2:
            return
        inproj(n, 0, 3072, 3584)
        P.add("act", lambda e: e.activation(out=avf[0:n, :], in_=PZ[0][0:n, :], func=AF.Copy), r=["PZ0"], w=["avf"])
        P.add("sp", lambda e: [e.dma_start(out=v_dst, in_=avf[0:n, :])], r=["avf"], dma="vst")
        if CUTB < 3:
            return
        inproj(n, 1, 3584, 4096)
        inproj(n, 2, 4096, 4168)
        if CUTB < 4:
            return
        if prompt:
            transposes(n, QK, ["QKa", "QKb"], 8)
            P.add("act", lambda e: e.activation(out=QKTe[0:64, 0:4, 0:n], in_=PTv[0:64, 0:4, 0:n], func=AF.Copy), r=["PT"], w=["QKT"])
            P.add("act", lambda e: e.activation(out=QKTo[64:128, 0:4, 0:n], in_=PTv[64:128, 0:4, 0:n], func=AF.Copy), r=["PT"], w=["QKT"])
            P.add("act", lambda e: e.activation(out=akT[:, :, r0:r0 + n], in_=PTv[:, 4:8, 0:n], func=AF.Copy), r=["PT"], w=["akT"])
            P.add("pool", lambda e: e.tensor_copy(out=Vaug[0:n, t, :, 0:64], in_=avf[0:n, :].rearrange("p (h d) -> p h d", d=64)),
                  r=["avf"], w=["Vaug"])
        if CUTB < 5:
            return
        P.add("act", lambda e: e.activation(out=ZA[0:n, 0:512], in_=PZ[1][0:n, :], func=AF.Copy), r=["PZ1"], w=["ZAa"])
        P.add("act", lambda e: e.activation(out=T1[0:n, 0:64], in_=PZ[2][0:n, 0:64], func=AF.Square, accum_out=st[0:n, 3:4]),
              r=["PZ2"], w=["T1", "st3"])
        P.add("act", lambda e: e.activation(out=st[0:n, 4:5], in_=st[0:n, 3:4], func=AF.Sqrt, bias=eps_t[0:n, :], scale=1.0 / 64),
              r=["st3", "eps"], w=["st4"])
        P.add("dve", lambda e: e.reciprocal(out=st[0:n, 5:6], in_=st[0:n, 4:5]), r=["st4"], w=["st5"])
        P.add("dve", lambda e: e.scalar_tensor_tensor(out=ZA[0:n, 512:576], in0=PZ[2][0:n, 0:64], scalar=st[0:n, 5:6], in1=iknw[0:n, :],
                                                      op0=ALU.mult, op1=ALU.mult), r=["PZ2", "st5", "iknw"], w=["ZAb"])
        P.add("dve", lambda e: e.tensor_scalar(out=wiw[0:n, :], in0=PZ[2][0:n, 64:72], scalar1=(8.0 ** -0.5) * 0.125, scalar2=None, op0=ALU.mult),
              r=["PZ2"], w=["wiw"])
        rope(n, ZA[0:n, 0:576], ZA[0:n, 0:576], 9, ["ZAa", "ZAb"], ["ZAa", "ZAb"])
        P.add("sp", lambda e: [e.dma_start(out=ik_dst, in_=ZA[0:n, 512:576])], r=["ZAb"], dma="ikst")
        if CUTB < 6:
            return
        if prompt:
            P.add("act", lambda e: e.activation(out=IQ[0:n, 0:576], in_=ZA[0:n, 0:576], func=AF.Copy), r=["ZAa", "ZAb"], w=["IQ"])
            P.add("pool", lambda e: e.tensor_copy(out=IQ[0:n, 576:640], in_=ZA[0:n, 512:576]), r=["ZAb"], w=["IQb"])
            transposes(n, IQ, ["IQ", "IQb"], 5)
            P.add("act", lambda e: e.activation(out=IQTe[0:64, :, 0:n], in_=PTv[0:64, 0:4, 0:n], func=AF.Copy), r=["PT"], w=["IQT"])
            P.add("act", lambda e: e.activation(out=IQTo[64:128, :, 0:n], in_=PTv[64:128, 0:4, 0:n], func=AF.Copy), r=["PT"], w=["IQT"])
            P.add("act", lambda e: e.activation(out=ikT2[:, r0:r0 + n], in_=PTv[:, 4, 0:n], func=AF.Copy), r=["PT"], w=["ikT2"])

    def dsa_prompt(t):
        nk = 128 * (t + 1)
        if t >= 2:
            ci = 0
            for c0 in range(0, nk, 512):
                w = min(512, nk - c0)
                for h in range(8):
                    p, base = h // 2, 64 * (h % 2)
                    pp, pk = (PA, "PA") if ci % 2 == 0 else (PB, "PB")
                    ci += 1
                    P.add("pe", lambda e, pp=pp, w=w, c0=c0, p=p, h=h: e.matmul(
                        pp[:, 0:w], lhsT=(IQTe if h % 2 == 0 else IQTo)[:, p, :], rhs=ikT2[:, c0:c0 + w], start=True, stop=True),
                        r=["IQT", "ikT2"], w=[pk])
                    P.add("act", lambda e, pp=pp, w=w: e.activation(out=relu_t[0][:, 0:w], in_=pp[:, 0:w], func=AF.Relu), r=[pk], w=["relu"])
                    if h == 0:
                        P.add("dve", lambda e, w=w, c0=c0, h=h: e.tensor_scalar(out=SC[:, c0:c0 + w], in0=relu_t[0][:, 0:w], scalar1=wiw[:, h:h + 1],
                                                                                scalar2=None, op0=ALU.mult), r=["relu", "wiw"], w=["SC"])
                    else:
                        P.add("dve", lambda e, w=w, c0=c0, h=h: e.scalar_tensor_tensor(out=SC[:, c0:c0 + w], in0=relu_t[0][:, 0:w], scalar=wiw[:, h:h + 1],
                                                                                       in1=SC[:, c0:c0 + w], op0=ALU.mult, op1=ALU.add),
                              r=["relu", "wiw", "SC"], w=["SC"])
            P.add("pool", lambda e: e.tensor_tensor(out=SC[:, nk - 128:nk], in0=SC[:, nk - 128:nk], in1=negdiag[:, :], op=ALU.add),
                  r=["SC", "negdiag"], w=["SC"])
            for rnd in range(32):
                P.add("dve", lambda e: e.max(out=m8[:, :], in_=SC[:, 0:nk]), r=["SC"], w=["m8"])
                P.add("dve", lambda e: e.match_replace(out=SC[:, 0:nk], in_to_replace=m8[:, :], in_values=SC[:, 0:nk], imm_value=REPL),
                      r=["SC", "m8"], w=["SC"])
            P.add("pool", lambda e: e.tensor_scalar(out=MASK[:, 0:nk], in0=SC[:, 0:nk], scalar1=REPL, scalar2=None, op0=ALU.is_equal),
                  r=["SC"], w=["MASK"])
            for k0 in range(0, t + 1, 8):
                kn = min(8, t + 1 - k0)

                def tr(e, k0=k0, kn=kn):
                    ins = None
                    for j in range(kn):
                        ins = e.transpose(out=PT[:, j * 128:(j + 1) * 128], in_=MASK[:, (k0 + j) * 128:(k0 + j + 1) * 128], identity=identb[:, :])
                    return ins
                P.add("pe", tr, r=["MASK", "identb"], w=["PT"])
                P.add("act", lambda e, k0=k0, kn=kn: e.activation(out=MT[:, k0:k0 + kn, :], in_=PTv[:, 0:kn, :], func=AF.Copy), r=["PT"], w=["MT"])
        else:
            if t == 1:
                P.add("pool", lambda e: e.memset(MT[:, 0, :], 1.0), w=["MT"])
            P.add("pool", lambda e: e.tensor_copy(out=MT[:, t, :], in_=causT[:, :]), r=["causT", "MT"], w=["MT"])
        gi = 0
        for h in range(8):
            p, base = h // 2, 64 * (h % 2)
            pout, pokey = (PO, "PO") if h < 4 else (PK, "PK")
            hh = h % 4
            for k0 in range(0, t + 1, 4):
                kn = min(4, t + 1 - k0)
                pp, pk = (PA, "PA") if gi % 2 == 0 else (PB, "PB")
                eb, ek = Eb[gi % 2], "Eb%d" % (gi % 2)
                gi += 1

                def fs(e, pp=pp, k0=k0, kn=kn, p=p, h=h):
                    ins = None
                    for j in range(kn):
                        ins = e.matmul(pp[:, j * 128:(j + 1) * 128], lhsT=akT[:, p, (k0 + j) * 128:(k0 + j + 1) * 128],
                                       rhs=(QKTe if h % 2 == 0 else QKTo)[:, p, :], start=True, stop=True)
                    return ins
                P.add("pe", fs, r=["akT", "QKT"], w=[pk])
                P.add("act", lambda e, pp=pp, eb=eb, kn=kn: e.activation(out=eb[:, 0:kn * 128], in_=pp[:, 0:kn * 128], func=AF.Exp, scale=0.125),
                      r=[pk], w=[ek])
                P.add("pool", lambda e, eb=eb, k0=k0, kn=kn: e.tensor_tensor(
                    out=eb[:, 0:kn * 128].rearrange("p (k q) -> p k q", q=128), in0=eb[:, 0:kn * 128].rearrange("p (k q) -> p k q", q=128),
                    in1=MT[:, k0:k0 + kn, :], op=ALU.mult), r=[ek, "MT"], w=[ek])

                def fv(e, eb=eb, k0=k0, kn=kn, pout=pout, hh=hh, h=h):
                    ins = None
                    for j in range(kn):
                        ins = e.matmul(pout[:, hh * 65:(hh + 1) * 65], lhsT=eb[:, j * 128:(j + 1) * 128], rhs=Vaug[:, k0 + j, h, 0:65],
                                       start=(k0 + j == 0), stop=(k0 + j == t))
                    return ins
                P.add("pe", fv, r=[ek, "Vaug"], w=[pokey])
        for bi, (pout, pokey) in enumerate(((PO, "PO"), (PK, "PK"))):
            pv = pout[:, 0:260].rearrange("p (h c) -> p h c", c=65)
            P.add("dve", lambda e, pv=pv, bi=bi: e.reciprocal(out=st[:, 56 + 4 * bi:60 + 4 * bi], in_=pv[:, :, 64]), r=[pokey], w=["st56_%d" % bi])
            P.add("dve", lambda e, pv=pv, bi=bi: e.tensor_tensor(
                out=mix[:, 512 + bi * 256:512 + (bi + 1) * 256].rearrange("p (h d) -> p h d", d=64), in0=pv[:, :, 0:64],
                in1=st[:, 56 + 4 * bi:60 + 4 * bi].unsqueeze(2).to_broadcast([128, 4, 64]), op=ALU.mult),
                r=[pokey, "st56_%d" % bi], w=["mixb%d" % bi])

    rwf_holder = {}

    def post(n, tile, mcol, Gx1, x1_dst, h2_dst):
        bc = (n == 128)

        def mview(Tt, lo, hi):
            v = Tt[:, lo:hi, mcol]
            return v.to_broadcast([128, hi - lo, n]) if bc else v
        transposes(n, mix, ["mixa", "mixb0", "mixb1"], 8)
        P.add("act", lambda e: e.activation(out=mixT[:, :, 0:n], in_=PTv[:, :, 0:n], func=AF.Copy), r=["PT"], w=["PTm0", "PTm1"])
        for half in range(2):
            pz = PZ[half]

            def f(e, half=half, pz=pz):
                ins = None
                for kc in range(8):
                    ins = e.matmul(pz[0:n, :], lhsT=mixT[:, kc, 0:n], rhs=w_out[:, kc, half * 512:(half + 1) * 512], start=(kc == 0), stop=(kc == 7))
                return ins
            P.add("pe", f, r=["PTm0", "PTm1", "w_out"], w=["PZ%d" % half])
            P.add("dve", lambda e, half=half, pz=pz: e.tensor_tensor(out=T1[0:n, half * 512:(half + 1) * 512], in0=pz[0:n, :],
                                                                    in1=Gx1[0:n, half * 512:(half + 1) * 512], op=ALU.mult),
                  r=["PZ%d" % half, "G1"], w=["T1"])
        P.add("pool", lambda e: e.tensor_tensor(out=xt[0:n, :], in0=xt[0:n, :], in1=T1[0:n, :], op=ALU.add), r=["xt", "T1"], w=["xt"])
        P.add("sp", lambda e: [e.dma_start(out=x1_dst, in_=xt[0:n, :])], r=["xt"], dma="x1st")
        P.add("act", lambda e: e.activation(out=xn[0:n, :], in_=xt[0:n, :], func=AF.Square, accum_out=st[0:n, 0:1]),
              r=["xt"], w=["xn", "st0"])
        P.add("act", lambda e: e.activation(out=st[0:n, 1:2], in_=st[0:n, 0:1], func=AF.Sqrt, bias=eps_t[0:n, :], scale=1.0 / D),
              r=["st0", "eps"], w=["st1"])
        P.add("dve", lambda e: e.reciprocal(out=st[0:n, 2:3], in_=st[0:n, 1:2]), r=["st1"], w=["st2"])
        P.add("act", lambda e: e.activation(out=xn[0:n, :], in_=xt[0:n, :], func=AF.Copy, scale=st[0:n, 2:3]),
              r=["xt", "st2"], w=["xn"])
        transpose_mod(n, xn, 8, A2T, 24, mview, hT, "hT")
        P.add("sp", lambda e: [e.dma_start(out=h2_dst, in_=hT[:, :, 0:n])], r=["hT"], dma="h2st")

        def fr(e):
            ins = None
            for kc in range(8):
                ins = e.matmul(PZ[2][0:n, 0:NE], lhsT=hT[:, kc, 0:n], rhs=rw[:, kc, :], start=(kc == 0), stop=(kc == 7))
            return ins
        if n == 128:
            P.add("pe", fr, r=["hT", "rw"], w=["PZ2"])
        else:
            rwf = rwf_holder["t"]
            P.add("act", lambda e: e.activation(out=T2[0:n, :], in_=xt[0:n, :], func=AF.Copy, scale=st[0:n, 2:3]),
                  r=["xt", "st2"], w=["T2a", "T2b"])

            def trf(e):
                ins = None
                for kc in range(8):
                    ins = e.transpose(out=PA[:, kc * n:(kc + 1) * n], in_=T2[0:n, kc * 128:(kc + 1) * 128], identity=identf[0:n, 0:n])
                return ins
            P.add("pe", trf, r=["T2a", "T2b", "identf"], w=["PA"])
            PAv = PA[:, 0:8 * n].rearrange("p (k t) -> p k t", t=n)
            h2f = T1[:, 512:512 + 8 * n].rearrange("p (k t) -> p k t", t=n)
            P.add("dve", lambda e: e.tensor_tensor(out=h2f, in0=PAv, in1=A2T[:, :, mcol], op=ALU.mult), r=["PA", "A2T"], w=["T1"])
            P.add("dve", lambda e: e.tensor_tensor(out=h2f, in0=h2f, in1=modT[:, 24:32, mcol], op=ALU.add), r=["T1", "modT"], w=["T1"])

            def frf(e):
                ins = None
                for kc in range(8):
                    ins = e.matmul(PZ[2][0:n, 0:NE], lhsT=h2f[:, kc, :], rhs=rwf[:, kc, :], start=(kc == 0), stop=(kc == 7))
                return ins
            P.add("pe", frf, r=["T1", "rwf"], w=["PZ2"])
        P.add("dve", lambda e: e.tensor_tensor(out=lg[0:n, :], in0=PZ[2][0:n, 0:NE], in1=rb_bc[0:n, :], op=ALU.add), r=["PZ2", "rb_bc"], w=["lg"])
        P.add("dve", lambda e: e.max(out=m8[0:n, :], in_=lg[0:n, :]), r=["lg"], w=["m8"])
        P.add("dve", lambda e: e.tensor_scalar(out=st[0:n, 6:7], in0=m8[0:n, 0:1], scalar1=-1.0, scalar2=None, op0=ALU.mult), r=["m8"], w=["st6"])
        P.add("act", lambda e: e.activation(out=lgw[0:n, :], in_=lg[0:n, :], func=AF.Exp, bias=st[0:n, 6:7], scale=1.0), r=["lg", "st6"], w=["lgw"])
        P.add("dve", lambda e: e.tensor_scalar(out=lg[0:n, :], in0=lg[0:n, :], scalar1=m8[0:n, 3:4], scalar2=None, op0=ALU.is_ge), r=["lg", "m8"], w=["lg"])
        P.add("dve", lambda e: e.tensor_tensor(out=lgw[0:n, :], in0=lgw[0:n, :], in1=lg[0:n, :], op=ALU.mult), r=["lgw", "lg"], w=["lgw"])
        P.add("dve", lambda e: e.tensor_reduce(out=st[0:n, 7:8], in_=lgw[0:n, :], axis=AX.X, op=ALU.add), r=["lgw"], w=["st7"])
        P.add("dve", lambda e: e.reciprocal(out=st[0:n, 6:7], in_=st[0:n, 7:8]), r=["st7"], w=["st6"])
        P.add("dve", lambda e: e.tensor_scalar(out=cw_all[0:n, tile, :], in0=lgw[0:n, :], scalar1=st[0:n, 6:7], scalar2=None, op0=ALU.mult),
              r=["lgw", "st6"], w=["cw"])

    esP = contextlib.ExitStack()
    TP = mk(esP)
    akT = TP("akT", [128, 4, SEQ], BF16)
    ikT2 = TP("ikT2", [128, SEQ], BF16)
    Vaug = TP("Vaug", [128, NT, 8, 66], BF16)
    SC = TP("SC", [128, SEQ])
    MASK = TP("MASK", [128, SEQ], BF16)
    MT = TP("MT", [128, NT, 128], BF16)
    Eb = [TP("Eb%d" % i, [128, 512], BF16) for i in range(2)]
    relu_t = [TP("relu%d" % i, [128, 512]) for i in range(1)]
    ld(gtab[:], I["g_p"], "gtab")
    P.add("pool", lambda e: e.memset(Vaug[:], 1.0), w=["Vaug"])
    for zt, zk in ((QKTe, "QKT"), (QKTo, "QKT"), (IQTe, "IQT"), (IQTo, "IQT")):
        P.add("pool", lambda e, zt=zt: e.memset(zt[:], 0.0), w=[zk])
    P.add("pool", lambda e: e.memset(S2[:], 0.0), w=["S2"])
    P.add("pool", lambda e: e.memset(S2b[:], 0.0), w=["S2b"])

    ntiles = int(os.environ.get('NTILES', '0')) or (NT if STAGE >= 2 else 2)
    for t in range(ntiles):
        r0 = t * 128
        premix_a(128, I["x_p"][r0:r0 + 128, :], slice(4, 5),
                 (I["cos_p"][r0:r0 + 128, :], I["sin_p"][r0:r0 + 128, :], I["qsc_p"], I["ksc_p"]))
        if CUT >= 2:
            retention_prompt(t)
        if CUT >= 3:
            premix_b(128, t, r0, O["k_p"][r0:r0 + 128, :], O["v_p"][r0:r0 + 128, :], O["ik_p"][r0:r0 + 128, :], True)
        if CUT >= 4:
            dsa_prompt(t)
        if CUT >= 5:
            post(128, t, slice(4, 5), G1bc, x1_scr[r0:r0 + 128, :], h2_scr[:, :, r0:r0 + 128])
        if STAGE < 5:
            P.add("sp", lambda e, r0=r0: [e.dma_start(out=O["y_p"][r0:r0 + 128, :], in_=xt[:, :])], r=["xt"], dma="x1st")
    if CUT >= 6:
      P.add("sp", lambda e: [e.dma_start(out=O["ret_p"].rearrange("(q par) d v -> (par d) q v", par=2), in_=S2[:, :, :])], r=["S2"], dma="retst")
    P.emit(last=(STAGE < 5))
    esP.close()
    if SAMPLE_ON:
        n = NS
        esS = contextlib.ExitStack()
        TS = mk(esS)
        selb = TS("selb", [NS, NS, 64], BF16)
        self_ = TS("self", [NS, NS, 64])
        tsel = TS("tsel", [128, NS, NS], BF16)
        esel = TS("esel", [8, NS, NS])
        bdiag = TS("bdiag", [8, 512])
        Gs1 = TS("Gs1", [NS, D])
        Gs2 = TS("Gs2", [NS, D])
        Qm = TS("Qm", [128, NS, 2, 4, NS], BF16)
        S2b4 = TS("S2b4", [128, NS, 4, 64], BF16)
        pt_i = TS("pt_i", [128, NS], I32)
        ptf = TS("ptf", [128, NS])
        idxf = TS("idxf", [128, NS, 32])
        c16f = TS("c16f", [64, 32])
        c16 = TS("c16", [64, 32], I32)
        idxc = TS("idxc", [128, NS, 32], I32)
        iqb = TS("iqb", [64, 512])
        wib = TS("wib", [64, 8])
        SCs = TS("SCs", [64, 128])
        MK = TS("MK", [64, 128])
        thrb = TS("thrb", [64, NS])
        ones64 = TS("ones64", [64, 1])
        sc8 = TS("sc8", [64, 4, 8])
        pe8 = TS("pe8", [64, 4, 8])
        nm = TS("nm", [8, 512])
        dd = TS("dd", [8, 8])
        dsb = TS("dsb", [8, 1])
        rwf = TS("rwf", [128, 8, NE])
        rwf_holder["t"] = rwf
        ld(rwf[:], I["router_w"].rearrange("(kc p) n -> p kc n", p=128), "rwf")
        P.add("pool", lambda e: [e.dma_start(out=selb[:], in_=I["selS"])], w=["selb"], dma="selb")
        P.add("pool", lambda e: [e.dma_start(out=tsel[:], in_=I["tsel"])], w=["tsel"], dma="tsel")
        ld(self_[:], I["selS"], "self")
        ld(esel[:], I["esel"], "esel")
        ld(bdiag[:], I["bdiag"], "bdiag")
        ld(c16[:], I["c16"], "c16")
        P.add("pool", lambda e: e.memset(idxf[:], 0.0), w=["idxf"])
        P.add("pool", lambda e: e.memset(ones64[:], 1.0), w=["ones64"])
        for s_ in range(NS):
            P.add("sp", lambda e, s_=s_: [e.dma_start(out=pt_i[0:64, s_:s_ + 1], in_=I["ptab"][s_:s_ + 1, :].rearrange("o j -> j o"), allow_slow_non_contiguous=True)],
                  w=["pt_i%d" % s_], dma="pti")
        PTI = ["pt_i%d" % k for k in range(NS)]
        P.add("dve", lambda e: e.tensor_copy(out=ptf[0:64, :], in_=pt_i[0:64, :]), r=PTI, w=["ptf"])
        P.add("dve", lambda e: e.tensor_copy(out=c16f[:, :], in_=c16[:, :]), r=["c16"], w=["c16f"])
        for s_ in range(NS):
            P.add("dve", lambda e, s_=s_: e.scalar_tensor_tensor(out=idxf[0:64, s_, :], in0=ptf[0:64, s_:s_ + 1].to_broadcast([64, 32]), scalar=32.0,
                                                              in1=c16f[:, :], op0=ALU.mult, op1=ALU.add),
                  r=["ptf", "c16f", "idxf"], w=["idxf%d" % s_])
        P.add("dve", lambda e: e.tensor_copy(out=idxc[:, :, :], in_=idxf[:, :, :]), r=["idxf%d" % k for k in range(NS)], w=["idxc%d" % k for k in range(NS)])
        IDXC = ["idxc%d" % k for k in range(NS)]
        for (vec, Gt, gk) in ((2, Gs1, "G1"), (5, Gs2, "Gs2")):
            for half in range(2):
                def trg(e, vec=vec, half=half):
                    ins = None
                    for j in range(4):
                        ch = half * 4 + j
                        ins = e.transpose(out=PZ[half][0:NS, j * 128:(j + 1) * 128], in_=modT[:, vec * 8 + ch, 0:NS], identity=identf[:, :])
                    return ins
                P.add("pe", trg, r=["modT", "identf"], w=["PZ%d" % half])
                P.add("act", lambda e, Gt=Gt, half=half: e.activation(out=Gt[0:NS, half * 512:(half + 1) * 512], in_=PZ[half][0:NS, :], func=AF.Copy),
                      r=["PZ%d" % half], w=[gk])
        P.add("sp", lambda e: [e.dma_start(out=gs_scr[:, :], in_=Gs2[:, :])], r=["Gs2"], dma="gsst")

        premix_a(n, I["x_s"], slice(0, 4), (I["cos_s"], I["sin_s"], I["qsc_s"], I["ksc_s"]))
        transposes(n, QK, ["QKa", "QKb"], 8)
        P.add("act", lambda e: e.activation(out=QKTe[0:64, :, 0:n], in_=PTv[0:64, :, 0:n], func=AF.Copy), r=["PT"], w=["QKT"])
        P.add("act", lambda e: e.activation(out=QKTo[64:128, :, 0:n], in_=PTv[64:128, :, 0:n], func=AF.Copy), r=["PT"], w=["QKT"])
        for s_ in range(NS):
            for par, Q in enumerate((QKTe, QKTo)):
                P.add("dve", lambda e, s_=s_, par=par, Q=Q: e.tensor_tensor(
                    out=Qm[:, s_, par, :, :], in0=Q[:, 0:4, 0:NS], in1=tsel[:, s_, :].unsqueeze(1).to_broadcast([128, 4, NS]), op=ALU.mult),
                    r=["QKT", "tsel"], w=["Qm%d%d" % (s_, par)])
        ld(m8[0:64, :], I["g_s2"], "m8")
        T1v = T1[0:64, 0:512].rearrange("p (h v) -> p h v", v=64)
        T2v = T2[0:64, 0:512].rearrange("p (h v) -> p h v", v=64)
        for s_ in range(NS):
            P.add("dve", lambda e, s_=s_: e.tensor_scalar(out=mix[0:n, 0:512], in0=QK[0:n, 512:1024], scalar1=identf[0:n, s_:s_ + 1],
                                                          scalar2=None, op0=ALU.mult), r=["QKb", "identf"], w=["mixa"])

            def fkv_s(e):
                ins = None
                for h in range(8):
                    ins = e.matmul(PO[0:64, h * 64:(h + 1) * 64], lhsT=mix[0:n, h * 64:(h + 1) * 64], rhs=vr[0:n, h * 64:(h + 1) * 64],
                                   start=True, stop=True)
                return ins
            P.add("pe", fkv_s, r=["mixa", "vr"], w=["PO"])
            P.add("sp", lambda e, s_=s_: [e.dma_start(out=T1v, in_=I["state"][s_].rearrange("h d v -> d h v"))], w=["T1"], dma="stld")
            P.add("dve", lambda e: e.tensor_tensor(out=T2[0:64, 0:512], in0=PO[0:64, :], in1=T1[0:64, 0:512], op=ALU.add),
                  r=["PO", "T1"], w=["T2a", "T2b"])
            P.add("pool", lambda e: e.tensor_tensor(out=T2v, in0=T2v, in1=m8[0:64, :].unsqueeze(2).to_broadcast([64, 8, 64]), op=ALU.mult),
                  r=["T2a", "T2b", "m8"], w=["T2a", "T2b"])
            P.add("sp", lambda e, s_=s_: [e.dma_start(out=O["ret_s"][s_].rearrange("h d v -> d h v"), in_=T2v)], r=["T2a", "T2b"], dma="retst")
            P.add("sp", lambda e, s_=s_: [e.dma_start(out=S2[:, :, :], in_=I["state"][s_].rearrange("(q par) d v -> (par d) q v", par=2))],
                  w=["S2"], dma="s2ld")
            P.add("pool", lambda e, s_=s_: e.tensor_copy(out=S2b4[:, s_, :, :], in_=S2[:]), r=["S2"], w=["S2b4_%d" % s_])

        def fro(e):
            ins = None
            for h in range(8):
                p, par = h // 2, h % 2
                for s_ in range(NS):
                    ins = e.matmul(PK[0:NS, h * 64:(h + 1) * 64], lhsT=Qm[:, s_, par, p, :], rhs=S2b4[:, s_, p, :],
                                   start=(s_ == 0), stop=(s_ == NS - 1))
            return ins
        P.add("pe", fro, r=["S2b4_%d" % k for k in range(NS)] + ["Qm%d%d" % (k, q) for k in range(NS) for q in range(2)], w=["PK"])
        P.add("dve", lambda e: e.tensor_tensor(out=T1[0:n, 0:512], in0=QK[0:n, 0:512], in1=QK[0:n, 512:1024], op=ALU.mult), r=["QKa", "QKb"], w=["T1"])
        P.add("dve", lambda e: e.tensor_reduce(out=st[0:n, 8:16], in_=T1[0:n, 0:512].rearrange("p (h d) -> p h d", d=64), axis=AX.X, op=ALU.add),
              r=["T1"], w=["st8"])
        P.add("pool", lambda e: e.tensor_tensor(out=T2[0:n, 0:512].rearrange("p (h d) -> p h d", d=64),
                                                in0=vr[0:n, :].rearrange("p (h d) -> p h d", d=64),
                                                in1=st[0:n, 8:16].unsqueeze(2).to_broadcast([n, 8, 64]), op=ALU.mult),
              r=["vr", "st8"], w=["T2a", "T2b"])
        P.add("dve", lambda e: e.tensor_tensor(out=T2[0:n, 0:512], in0=PK[0:n, :], in1=T2[0:n, 0:512], op=ALU.add), r=["PK", "T2a", "T2b"], w=["T2a", "T2b"])
        groupnorm_gate(n, ["T2a", "T2b"], src=T2[0:n, 0:512])
        premix_b(n, None, None, O["k_s"], O["v_s"], O["ik_s"], False)
        P.add("act", lambda e: e.activation(out=IQ[0:n, 0:512], in_=ZA[0:n, 0:512], func=AF.Copy), r=["ZAa"], w=["IQ"])
        P.add("dve", lambda e: e.tensor_tensor(out=T1[0:n, 0:512].rearrange("p (h d) -> p h d", d=64),
                                               in0=ZA[0:n, 0:512].rearrange("p (h d) -> p h d", d=64),
                                               in1=ZA[0:n, 512:576].unsqueeze(1).to_broadcast([n, 8, 64]), op=ALU.mult),
              r=["ZAa", "ZAb"], w=["T1"])
        P.add("dve", lambda e: e.tensor_reduce(out=st[0:n, 16:24], in_=T1[0:n, 0:512].rearrange("p (h d) -> p h d", d=64), axis=AX.X, op=ALU.add),
              r=["T1"], w=["st16"])
        P.add("dve", lambda e: e.tensor_scalar(out=st[0:n, 16:24], in0=st[0:n, 16:24], scalar1=0.0, scalar2=None, op0=ALU.max), r=["st16"], w=["st16"])
        P.add("dve", lambda e: e.tensor_tensor(out=st[0:n, 24:32], in0=st[0:n, 16:24], in1=wiw[0:n, :], op=ALU.mult), r=["st16", "wiw"], w=["st24"])
        P.add("dve", lambda e: e.tensor_reduce(out=st[0:n, 32:33], in_=st[0:n, 24:32], axis=AX.X, op=ALU.add), r=["st24"], w=["st32"])
        P.add("sp", lambda e: [e.dma_start(out=sc_scr[0:n, 8192:8193], in_=st[0:n, 32:33], allow_slow_non_contiguous=True)], r=["st32"], w=["scscr"], dma="scst")
        P.emit()
        es1 = contextlib.ExitStack()
        T_1 = mk(es1)
        KI = T_1("KI", [64, 128, 64])
        tmpm = T_1("tmpm", [64, 32, 64])
        red = T_1("red", [64, 32])
        for s_ in range(NS):
            P.add("pool", lambda e, s_=s_: [e.indirect_dma_start(out=KI[:, :, :].rearrange("p r d -> p (r d)"), out_offset=None, in_=I["cache_ik"][:, :],
                                                               in_offset=bass.IndirectOffsetOnAxis(ap=pt_i[0:64, s_:s_ + 1], axis=0))],
                  r=PTI, w=["KI"], dma="KI")
            P.add("pe", lambda e, s_=s_: e.matmul(PA[0:64, 0:512], lhsT=self_[:, s_, :], rhs=ZA[0:n, 0:512], start=True, stop=True), r=["self", "ZAa"], w=["PA"])
            P.add("pe", lambda e, s_=s_: e.matmul(PB[0:64, 0:8], lhsT=self_[:, s_, :], rhs=wiw[0:n, :], start=True, stop=True), r=["self", "wiw"], w=["PB"])
            P.add("act", lambda e: e.activation(out=iqb[:, :], in_=PA[0:64, 0:512], func=AF.Copy), r=["PA"], w=["iqb"])
            P.add("act", lambda e: e.activation(out=wib[:, :], in_=PB[0:64, 0:8], func=AF.Copy), r=["PB"], w=["wib"])
            for half in range(4):
                for h in range(8):
                    P.add("pool", lambda e, half=half, h=h: e.tensor_tensor(
                        out=tmpm[:, :, :], in0=KI[:, half * 32:(half + 1) * 32, :],
                        in1=iqb[:, h * 64:(h + 1) * 64].unsqueeze(1).to_broadcast([64, 32, 64]), op=ALU.mult), r=["KI", "iqb"], w=["tmpm"])
                    P.add("dve", lambda e: e.tensor_reduce(out=red[:, :], in_=tmpm[:, :, :], axis=AX.X, op=ALU.add), r=["tmpm"], w=["red"])
                    P.add("dve", lambda e, h=h: e.tensor_scalar(out=red[:, :], in0=red[:, :], scalar1=0.0, scalar2=wib[:, h:h + 1], op0=ALU.max, op1=ALU.mult),
                          r=["red", "wib"], w=["red"])
                    if h == 0:
                        P.add("dve", lambda e, half=half: e.tensor_copy(out=SCs[:, half * 32:(half + 1) * 32], in_=red[:, :]), r=["red"], w=["SCs"])
                    else:
                        P.add("dve", lambda e, half=half: e.tensor_tensor(out=SCs[:, half * 32:(half + 1) * 32], in0=SCs[:, half * 32:(half + 1) * 32],
                                                                          in1=red[:, :], op=ALU.add), r=["red", "SCs"], w=["SCs"])
            P.add("sp", lambda e, s_=s_: [e.dma_start(out=sc_scr[s_, 0:8192].rearrange("(j r) -> j r", r=128), in_=SCs[:, :])], r=["SCs"], w=["scscr"], dma="scst")
        P.emit()
        es1.close()
        es2 = contextlib.ExitStack()
        T_2 = mk(es2)
        ROW = T_2("ROW", [NS, 8200])
        ld(ROW[:, 0:8193], sc_scr[:, 0:8193], "ROW")
        P.add("dve", lambda e: e.tensor_copy(out=st[0:n, 34:35], in_=ROW[0:n, 8192:8193]), r=["ROW"], w=["st34"])
        for rnd in range(32):
            P.add("dve", lambda e: e.max(out=m8[0:n, :], in_=ROW[0:n, 0:8193]), r=["ROW"], w=["m8"])
            if rnd < 31:
                P.add("dve", lambda e: e.match_replace(out=ROW[0:n, 0:8193], in_to_replace=m8[0:n, :], in_values=ROW[0:n, 0:8193], imm_value=REPL),
                      r=["ROW", "m8"], w=["ROW"])
        P.add("sp", lambda e: [e.dma_start(out=thr_scr.rearrange("o s -> s o"), in_=m8[0:n, 7:8], allow_slow_non_contiguous=True)], r=["m8"], w=["thrscr"], dma="thrst")
        P.add("dve", lambda e: e.tensor_tensor(out=st[0:n, 33:34], in0=st[0:n, 34:35], in1=m8[0:n, 7:8], op=ALU.is_ge), r=["st34", "m8"], w=["st33"])
        P.add("sp", lambda e: [e.dma_start(out=thrb[:, :], in_=thr_scr[0:1, :].partition_broadcast(64))], r=["thrscr"], w=["thrb"], dma="thrb")
        P.emit()
        es2.close()
        es3 = contextlib.ExitStack()
        T_3 = mk(es3)
        Kc = [T_3("Kc%d" % i, [64, 4, 512]) for i in range(1)]
        Vc = [T_3("Vc%d" % i, [64, 4, 512]) for i in range(1)]
        stmp = T_3("stmp", [64, 4, 512])
        aqb = T_3("aqb", [64, 512])
        for s_ in range(NS):
            P.add("sp", lambda e, s_=s_: [e.dma_start(out=SCs[:, :], in_=sc_scr[s_, 0:8192].rearrange("(j r) -> j r", r=128))], r=["scscr"], w=["SCs"], dma="scld")
            P.add("dve", lambda e, s_=s_: e.tensor_scalar(out=MK[:, :], in0=SCs[:, :], scalar1=thrb[:, s_:s_ + 1], scalar2=None, op0=ALU.is_ge),
                  r=["SCs", "thrb"], w=["MK"])
            P.add("pe", lambda e, s_=s_: e.matmul(PA[0:64, 0:512], lhsT=selb[:, s_, :], rhs=QK[0:n, 0:512], start=True, stop=True), r=["selb", "QKa"], w=["PA"])
            P.add("act", lambda e: e.activation(out=aqb[:, :], in_=PA[0:64, 0:512], func=AF.Copy), r=["PA"], w=["aqb"])
            for c in range(32):
                P.add("pool", lambda e, s_=s_, c=c: [e.indirect_dma_start(out=Kc[0][:, :, :].rearrange("p r d -> p (r d)"), out_offset=None, in_=I["cache_k"][:, :],
                                                                       in_offset=bass.IndirectOffsetOnAxis(ap=idxc[0:64, s_, c:c + 1], axis=0))],
                      r=IDXC, w=["Kc"], dma="Kc")
                P.add("pool", lambda e, s_=s_, c=c: [e.indirect_dma_start(out=Vc[0][:, :, :].rearrange("p r d -> p (r d)"), out_offset=None, in_=I["cache_v"][:, :],
                                                                       in_offset=bass.IndirectOffsetOnAxis(ap=idxc[0:64, s_, c:c + 1], axis=0))],
                      r=IDXC, w=["Vc"], dma="Vc")
                P.add("pool", lambda e: e.tensor_tensor(out=stmp[:, :, :], in0=Kc[0][:, :, :], in1=aqb[:, :].unsqueeze(1).to_broadcast([64, 4, 512]), op=ALU.mult),
                      r=["Kc", "aqb"], w=["stmp"])
                P.add("dve", lambda e: e.tensor_reduce(out=sc8[:, :, :].rearrange("p r h -> p (r h)"), in_=stmp[:, :, :].rearrange("p r (h d) -> p (r h) d", d=64),
                                                       axis=AX.X, op=ALU.add), r=["stmp"], w=["sc8"])
                P.add("act", lambda e: e.activation(out=pe8[:, :, :], in_=sc8[:, :, :], func=AF.Exp, scale=0.125), r=["sc8"], w=["pe8"])
                P.add("dve", lambda e, c=c: e.tensor_tensor(out=pe8[:, :, :], in0=pe8[:, :, :], in1=MK[:, c * 4:(c + 1) * 4].unsqueeze(2).to_broadcast([64, 4, 8]), op=ALU.mult),
                      r=["pe8", "MK"], w=["pe8"])

                def fav(e, c=c):
                    ins = None
                    for r_ in range(4):
                        e.matmul(PO[0:8, 0:512], lhsT=pe8[:, r_, :], rhs=Vc[0][:, r_, :], start=(c == 0 and r_ == 0), stop=(c == 31 and r_ == 3))
                        ins = e.matmul(PB[0:8, 0:1], lhsT=pe8[:, r_, :], rhs=ones64[:, :], start=(c == 0 and r_ == 0), stop=(c == 31 and r_ == 3))
                    return ins
                P.add("pe", fav, r=["pe8", "Vc", "ones64"], w=["PO", "PB"])
            P.add("dve", lambda e: e.tensor_tensor(out=nm[:, :], in0=PO[0:8, :], in1=bdiag[:, :], op=ALU.mult), r=["PO", "bdiag"], w=["nm"])
            P.add("act", lambda e: e.activation(out=dsb[:, :], in_=PB[0:8, 0:1], func=AF.Copy), r=["PB"], w=["dsb"])
            P.add("dve", lambda e: e.tensor_scalar(out=dd[:, :], in0=identf[0:8, 0:8], scalar1=dsb[:, 0:1], scalar2=None, op0=ALU.mult), r=["dsb", "identf"], w=["dd"])
            P.add("pe", lambda e, s_=s_: e.matmul(PZ[0][0:NS, 0:512], lhsT=esel[:, s_, :], rhs=nm[:, :], start=(s_ == 0), stop=(s_ == NS - 1)),
                  r=["esel", "nm"], w=["PZ0"])
            P.add("pe", lambda e, s_=s_: e.matmul(PZ[1][0:NS, 0:8], lhsT=esel[:, s_, :], rhs=dd[:, :], start=(s_ == 0), stop=(s_ == NS - 1)),
                  r=["esel", "dd"], w=["PZ1"])
        P.add("dve", lambda e: e.tensor_tensor(out=T1[0:n, 0:512], in0=QK[0:n, 0:512], in1=QK[0:n, 512:1024], op=ALU.mult), r=["QKa", "QKb"], w=["T1"])
        P.add("dve", lambda e: e.tensor_reduce(out=st[0:n, 40:48], in_=T1[0:n, 0:512].rearrange("p (h d) -> p h d", d=64), axis=AX.X, op=ALU.add),
              r=["T1"], w=["st40"])
        P.add("act", lambda e: e.activation(out=st[0:n, 48:56], in_=st[0:n, 40:48], func=AF.Exp, scale=0.125), r=["st40"], w=["st48"])
        P.add("dve", lambda e: e.tensor_scalar(out=st[0:n, 48:56], in0=st[0:n, 48:56], scalar1=st[0:n, 33:34], scalar2=None, op0=ALU.mult),
              r=["st48", "st33"], w=["st48"])
        P.add("pool", lambda e: e.tensor_tensor(out=T2[0:n, 0:512].rearrange("p (h d) -> p h d", d=64),
                                                in0=avf[0:n, :].rearrange("p (h d) -> p h d", d=64),
                                                in1=st[0:n, 48:56].unsqueeze(2).to_broadcast([n, 8, 64]), op=ALU.mult),
              r=["avf", "st48"], w=["T2a", "T2b"])
        P.add("dve", lambda e: e.tensor_tensor(out=T2[0:n, 0:512], in0=PZ[0][0:n, :], in1=T2[0:n, 0:512], op=ALU.add), r=["PZ0", "T2a", "T2b"], w=["T2a", "T2b"])
        P.add("dve", lambda e: e.tensor_tensor(out=st[0:n, 56:64], in0=PZ[1][0:n, 0:8], in1=st[0:n, 48:56], op=ALU.add), r=["PZ1", "st48"], w=["st56"])
        P.add("dve", lambda e: e.reciprocal(out=st[0:n, 40:48], in_=st[0:n, 56:64]), r=["st56"], w=["st40"])
        P.add("dve", lambda e: e.tensor_tensor(out=mix[0:n, 512:1024].rearrange("p (h d) -> p h d", d=64),
                                               in0=T2[0:n, 0:512].rearrange("p (h d) -> p h d", d=64),
                                               in1=st[0:n, 40:48].unsqueeze(2).to_broadcast([n, 8, 64]), op=ALU.mult),
              r=["T2a", "T2b", "st40"], w=["mixb0", "mixb1"])
        post(n, NT, slice(0, 4), Gs1, x1_scr[SEQ:SEQ + n, :], h2_scr[:, :, SEQ:SEQ + n])
        P.emit()
        es3.close()
        esS.close()

    esB.close()
    if STAGE >= 5:
        moe_phase(nc, P, mk, I, O, ntiles, h2_scr, x1_scr, cw_all, G2bc, identf,
                  PZ, PA, PB, PO, PK, SAMPLE_ON, gs_scr)
    es0.close()
    return nc


def moe_phase(nc, P, mk, I, O, ntiles, h2_scr, x1_scr, cw_all, G2bc, identf, PZ, PA, PB, PO, PK, sample_on, gs_scr):
    esC = contextlib.ExitStack()
    TC = mk(esC)
    NTOK = SEQ + NS
    ntok_p = ntiles * 128
    tiles = [(t, t * 128, 128) for t in range(ntiles)]
    blocks = []
    c = 0
    while c < ntok_p:
        w = min(512, ntok_p - c)
        blocks.append((c, w))
        c += w
    if sample_on:
        tiles.append((NT, SEQ, NS))
        blocks.append((SEQ, NS))
    h2T = TC("h2T", [128, 8, NTOK], BF16)
    acc = TC("acc", [128, NT + 1, D])
    actT = TC("actT", [128, 8, NTOK], BF16)
    bgu = TC("bgu", [128, NE, 16])
    bdn = TC("bdn", [NE, D])
    esW = contextlib.ExitStack()
    TW = mk(esW)
    gu = TW("gu", [128, 8, 2 * D], BF16)
    dn = TW("dn", [128, 8, D], BF16)
    tmp3 = TW("tmp3", [128, 3, 512])
    gt = [tmp3[:, 0, :]] * 2
    gs = [tmp3[:, 1, :]] * 2
    ut = [tmp3[:, 2, :]] * 2

    def ld(dst, src, key, eng="sp"):
        P.add(eng, lambda e: [e.dma_start(out=dst, in_=src)], w=[key], dma=key)
    ld(h2T[:, :, 0:ntok_p], h2_scr[:, :, 0:ntok_p], "h2T")
    if sample_on:
        ld(h2T[:, :, SEQ:NTOK], h2_scr[:, :, SEQ:NTOK], "h2T")
    ld(bgu[:], I["bguT"], "bgu")
    ld(bdn[:], I["b_dn"], "bdn")
    P.add("dve", lambda en: en.tensor_scalar(out=cw_all[:, 0:ntiles, :], in0=cw_all[:, 0:ntiles, :], scalar1=1.0 / 1.702, scalar2=None, op0=ALU.mult), r=["cw"], w=["cw"])
    if sample_on:
        P.add("dve", lambda en: en.tensor_scalar(out=cw_all[0:NS, NT, :], in0=cw_all[0:NS, NT, :], scalar1=1.0 / 1.702, scalar2=None, op0=ALU.mult), r=["cw"], w=["cw"])
    P.add("dve", lambda en: en.tensor_scalar(out=bdn[:], in0=bdn[:], scalar1=1.702, scalar2=None, op0=ALU.mult), r=["bdn"], w=["bdn"])
    gu_v = I["w_gu"].rearrange("e (kc p) n -> e p kc n", p=128)
    dn_v = I["w_dn"].rearrange("e (kc p) n -> e p kc n", p=128)
    GUK = ["gu%d" % k for k in range(8)]
    DNK = ["dn%d" % k for k in range(8)]

    def load_gu(e):
        for kc in range(8):
            P.add("pool", lambda en, e=e, kc=kc: [en.dma_start(out=gu[:, kc, :], in_=gu_v[e, :, kc, :])], w=[GUK[kc]], dma=GUK[kc])

    def load_dn(e):
        for kc in range(8):
            P.add("pool", lambda en, e=e, kc=kc: [en.dma_start(out=dn[:, kc, :], in_=dn_v[e, :, kc, :])], w=[DNK[kc]], dma=DNK[kc])

    pairs = [(PZ[0], "PZ0", PZ[1], "PZ1"), (PZ[2], "PZ2", PA, "PA"), (PB, "PB", PO, "PO")]
    ui = 0
    load_gu(0)
    load_dn(0)
    for e in range(NEXP):
        for (c0, w) in blocks:
            for j in range(8):
                pg, pgk, pu, puk = pairs[ui % 3]
                b = 0
                ui += 1
                for (pp, ppk, col) in ((pg, pgk, j * 128), (pu, puk, D + j * 128)):
                    def mm(en, pp=pp, col=col, c0=c0, w=w):
                        ins = None
                        for kc in range(8):
                            ins = en.matmul(pp[:, 0:w], lhsT=gu[:, kc, col:col + 128], rhs=h2T[:, kc, c0:c0 + w],
                                            start=(kc == 0), stop=(kc == 7))
                        return ins
                    P.add("pe", mm, r=GUK + ["h2T"], w=[ppk])
                P.add("dve", lambda en, pg=pg, b=b, w=w, e=e, j=j: en.tensor_scalar(
                    out=gt[b][:, 0:w], in0=pg[:, 0:w], scalar1=bgu[:, e, j:j + 1], scalar2=7.0, op0=ALU.add, op1=ALU.min),
                    r=[pgk, "bgu"], w=["gt%d" % b])
                P.add("act", lambda en, b=b, w=w: en.activation(out=gs[b][:, 0:w], in_=gt[b][:, 0:w], func=AF.Silu, scale=1.702),
                      r=["gt%d" % b], w=["gs%d" % b])
                P.add("dve", lambda en, pu=pu, b=b, w=w, e=e, j=j: en.tensor_scalar(
                    out=ut[b][:, 0:w], in0=pu[:, 0:w], scalar1=bgu[:, e, 8 + j:9 + j], scalar2=7.0, op0=ALU.add, op1=ALU.min),
                    r=[puk, "bgu"], w=["ut%d" % b])
                P.add("dve", lambda en, b=b, w=w: en.tensor_scalar(
                    out=ut[b][:, 0:w], in0=ut[b][:, 0:w], scalar1=-7.0, scalar2=1.0, op0=ALU.max, op1=ALU.add),
                    r=["ut%d" % b], w=["ut%d" % b])
                P.add("pool", lambda en, b=b, w=w, j=j, c0=c0: en.tensor_tensor(
                    out=actT[:, j, c0:c0 + w], in0=ut[b][:, 0:w], in1=gs[b][:, 0:w], op=ALU.mult),
                    r=["ut%d" % b, "gs%d" % b], w=["actT"])
        if e + 1 < NEXP:
            load_gu(e + 1)
        for (t, tok0, n) in tiles:
            p0, p0k, p1, p1k = pairs[ui % 3]
            ui += 1
            for half, (pd, pdk) in enumerate(((p0, p0k), (p1, p1k))):
                def md(en, pd=pd, half=half, tok0=tok0, n=n):
                    ins = None
                    for fc in range(8):
                        ins = en.matmul(pd[0:n, :], lhsT=actT[:, fc, tok0:tok0 + n], rhs=dn[:, fc, half * 512:(half + 1) * 512],
                                        start=(fc == 0), stop=(fc == 7))
                    return ins
                P.add("pe", md, r=DNK + ["actT"], w=[pdk])
                av = acc[0:n, t, half * 512:(half + 1) * 512]
                if e == 0:
                    P.add("dve", lambda en, pd=pd, av=av, n=n, t=t, e=e: en.tensor_scalar(
                        out=av, in0=pd[0:n, :], scalar1=cw_all[0:n, t, e:e + 1], scalar2=None, op0=ALU.mult),
                        r=[pdk, "cw"], w=["acc%d_%d" % (t, half)])
                else:
                    P.add("dve", lambda en, pd=pd, av=av, n=n, t=t, e=e: en.scalar_tensor_tensor(
                        out=av, in0=pd[0:n, :], scalar=cw_all[0:n, t, e:e + 1], in1=av, op0=ALU.mult, op1=ALU.add),
                        r=[pdk, "cw", "acc%d_%d" % (t, half)], w=["acc%d_%d" % (t, half)])
        if e + 1 < NEXP:
            load_dn(e + 1)
    P.emit()
    esW.close()
    xo = TC("xo", [128, D])
    yo = TC("yo", [128, D])
    cwT = TC("cwT", [NE, 128])
    gs2 = TC("gs2", [NS, D])
    if sample_on:
        P.add("sp", lambda en: [en.dma_start(out=gs2[:, :], in_=gs_scr[:, :])], w=["G2bc"], dma="gs2")
    for (t, tok0, n) in tiles:
        P.add("pe", lambda en, n=n, t=t: en.transpose(out=PK[0:NE, 0:n], in_=cw_all[0:n, t, :], identity=identf[0:n, 0:n]),
              r=["cw", "identf"], w=["PK"])
        P.add("act", lambda en, n=n: en.activation(out=cwT[:, 0:n], in_=PK[0:NE, 0:n], func=AF.Copy), r=["PK"], w=["cwT"])
        P.add("sp", lambda en, n=n, tok0=tok0: [en.dma_start(out=xo[0:n, :], in_=x1_scr[tok0:tok0 + n, :])], w=["xo"], dma="xo")
        for half, (pp, ppk) in enumerate(((PA, "PA"), (PB, "PB"))):
            P.add("pe", lambda en, pp=pp, half=half, n=n: en.matmul(pp[0:n, :], lhsT=cwT[:, 0:n], rhs=bdn[:, half * 512:(half + 1) * 512],
                                                                   start=True, stop=True), r=["cwT", "bdn"], w=[ppk])
            P.add("dve", lambda en, pp=pp, half=half, n=n, t=t: en.tensor_tensor(
                out=yo[0:n, half * 512:(half + 1) * 512], in0=pp[0:n, :], in1=acc[0:n, t, half * 512:(half + 1) * 512], op=ALU.add),
                r=[ppk, "acc%d_%d" % (t, half)], w=["yo%d" % half])
        Gt = G2bc if t < NT else gs2
        P.add("pool", lambda en, n=n, Gt=Gt: en.tensor_tensor(out=yo[0:n, :], in0=yo[0:n, :], in1=Gt[0:n, :], op=ALU.mult),
              r=["yo0", "yo1", "G2bc"], w=["yo0", "yo1"])
        P.add("dve", lambda en, n=n: en.tensor_tensor(out=yo[0:n, :], in0=yo[0:n, :], in1=xo[0:n, :], op=ALU.add),
              r=["yo0", "yo1", "xo"], w=["yo0", "yo1"])
        dst = O["y_p"][tok0:tok0 + n, :] if t < NT else O["y_s"][0:n, :]
        P.add("sp", lambda en, n=n, dst=dst: [en.dma_start(out=dst, in_=yo[0:n, :])], r=["yo0", "yo1"], dma="yst")
    P.emit(last=True)
    esC.close()


_NC_CACHE = {}


def _get_nc():
    if "nc" not in _NC_CACHE:
        _NC_CACHE["nc"] = build_nc()
    return _NC_CACHE["nc"]


def kernel(x_prompt, x_sample, cache_k, cache_v, cache_idx_k, state_ret, page_table,
           c_prompt, c_sample, ada_w, ada_b, norm1_w, w_in, q_norm_w, k_norm_w,
           idx_k_norm_w, ret_gn_w, w_out, norm2_w, router_w, router_b,
           w_gate_up, b_gate_up, w_down, b_down):
    f = lambda a: np.ascontiguousarray(np.asarray(a))
    consts = _consts()
    shared = {
        "ada_w": f(ada_w[0]),
        "ada_bT": f(np.asarray(ada_b[0]).reshape(48, 128).T),
        "n1T": f(np.asarray(norm1_w[0]).reshape(8, 128).T),
        "n2T": f(np.asarray(norm2_w[0]).reshape(8, 128).T),
        "w_in": f(w_in[0]),
        "qknw": f(np.concatenate([np.asarray(q_norm_w[0]), np.asarray(k_norm_w[0])])[None, :]),
        "iknw": f(np.asarray(idx_k_norm_w[0])[None, :]),
        "gnw": f(np.asarray(ret_gn_w[0])[None, :]),
        "w_out": f(w_out[0]),
        "router_w": f(router_w[0]),
        "router_b": f(np.asarray(router_b[0])[None, :]),
        "w_gu": f(w_gate_up[0]),
        "bguT": f(np.asarray(b_gate_up[0]).reshape(NE, 16, 128).transpose(2, 0, 1)),
        "w_dn": f(w_down[0]),
        "b_dn": f(b_down[0]),
    }
    shared["cache_k"] = f(np.asarray(cache_k[0]).reshape(NPOOL_PAGES * 32, 2048))
    shared["cache_v"] = f(np.asarray(cache_v[0]).reshape(NPOOL_PAGES * 32, 2048))
    shared["cache_ik"] = f(np.asarray(cache_idx_k[0]).reshape(NPOOL_PAGES, 8192))
    shared["c16"] = np.ascontiguousarray(np.repeat(np.arange(32, dtype=np.int32)[None, :], 64, 0))
    shared.update(consts)
    in_maps = []
    ncores = 1 if DBG else NCORES
    for c in range(ncores):
        s0 = c * NS
        call = np.concatenate([np.asarray(c_sample[s0:s0 + NS]), np.asarray(c_prompt[c:c + 1])], 0)
        m = dict(shared)
        m["x_p"] = f(x_prompt[c])
        m["x_s"] = f(np.asarray(x_sample[s0:s0 + NS, 0]))
        m["cT"] = f(call.T.reshape(8, 128, 5).transpose(1, 0, 2))
        m["state"] = f(state_ret[0, s0:s0 + NS])
        m["ptab"] = f(np.asarray(page_table[s0:s0 + NS]).astype(np.int32))
        in_maps.append(m)
    nc = _get_nc()
    res = run_bass_kernel_spmd(nc, in_maps, core_ids=list(range(ncores)))
    R = res.results
    cat = lambda k: np.stack([np.asarray(R[min(c, ncores - 1)][k]) for c in range(NCORES)], 0)
    y_p = cat("y_p")
    y_s = cat("y_s").reshape(32, 1, D)
    k_p = cat("k_p").reshape(1, 8, SEQ, 8, 64)
    v_p = cat("v_p").reshape(1, 8, SEQ, 8, 64)
    ik_p = cat("ik_p").reshape(1, 8, SEQ, 64)
    ret_p = cat("ret_p").reshape(1, 8, 8, 64, 64)
    k_s = cat("k_s").reshape(1, 32, 1, 8, 64)
    v_s = cat("v_s").reshape(1, 32, 1, 8, 64)
    ik_s = cat("ik_s").reshape(1, 32, 1, 64)
    ret_s = cat("ret_s").reshape(1, 32, 8, 64, 64)
    return (y_p, y_s, k_p, v_p, ik_p, ret_p, k_s, v_s, ik_s, ret_s)
```
